# Optimizing a Trainium2 kernel written in Bass

```python
import math
import jax, jax.numpy as jnp
from jax import lax
import numpy as np

D_MODEL = 1024
BATCH = 4
SEQ = 4096
DEPTH = 1

N_HEADS = 4
HEAD_DIM = 64
V_DIM = 2 * HEAD_DIM
ATT_QK_WIDTH = N_HEADS * 2 * HEAD_DIM
ATT_V_WIDTH = N_HEADS * V_DIM
ROPE_THETA = 10000.0
Q_BLOCK = 128
CONV_WIDTH = D_MODEL // 2
CONV_K = 3
N_BRANCHES = 2
IN_PROJ_WIDTH = 3 * ATT_QK_WIDTH + 3 * CONV_WIDTH + N_BRANCHES * D_MODEL
N_GROUPS = 4
EXPERTS_PER_GROUP = 8
N_EXPERTS = N_GROUPS * EXPERTS_PER_GROUP
TOP_K = 2
EXPERT_FF = D_MODEL // 4
EPS = 1e-6
MASK_VALUE = -1e30

kernel_name = "hybrid_diffattn_shortconv_hmoe_block"


def rms_norm(x, g):
    xf = x.astype(jnp.float32)
    y = xf * lax.rsqrt(jnp.mean(xf * xf, axis=-1, keepdims=True) + EPS)
    return (y * g.astype(jnp.float32)).astype(x.dtype)


def rope(t, positions):
    inv_freq = ROPE_THETA ** (-jnp.arange(0, HEAD_DIM, 2, dtype=jnp.float32) / HEAD_DIM)
    ang = positions.astype(jnp.float32)[..., None] * inv_freq
    cos = jnp.cos(ang)[:, :, None, None, :].astype(t.dtype)
    sin = jnp.sin(ang)[:, :, None, None, :].astype(t.dtype)
    t1, t2 = jnp.split(t, 2, axis=-1)
    return jnp.concatenate([t1 * cos - t2 * sin, t2 * cos + t1 * sin], axis=-1)


def diff_attention(q, k, v, lam):
    B, S = q.shape[0], q.shape[1]
    n_blocks = S // Q_BLOCK
    scale = HEAD_DIM ** -0.5
    qb = q.reshape(B, n_blocks, Q_BLOCK, N_HEADS, 2, HEAD_DIM).transpose(1, 0, 2, 3, 4, 5)
    k_idx = jnp.arange(S)

    def one_block(args):
        q_blk, b_idx = args
        s = jnp.einsum('bqhmd,bkhmd->bhmqk', q_blk, k).astype(jnp.float32) * scale
        q_idx = b_idx * Q_BLOCK + jnp.arange(Q_BLOCK)
        causal = k_idx[None, :] <= q_idx[:, None]
        s = jnp.where(causal, s, MASK_VALUE)
        p = jax.nn.softmax(s, axis=-1)
        a = p[:, :, 0] - lam * p[:, :, 1]
        return jnp.einsum('bhqk,bkhv->bqhv', a.astype(v.dtype), v)

    out = lax.map(one_block, (qb, jnp.arange(n_blocks)))
    return out.transpose(1, 0, 2, 3, 4).reshape(B, S, N_HEADS, V_DIM)


def causal_short_conv(u, w):
    S = u.shape[1]
    up = jnp.pad(u, ((0, 0), (CONV_K - 1, 0), (0, 0)))
    return sum(w[j] * up[:, CONV_K - 1 - j: CONV_K - 1 - j + S] for j in range(CONV_K))


def hierarchical_moe(t, w_grp, b_grp, w_exp_r, b_exp_r, w_gate, w_up, w_down):
    T = t.shape[0]
    g_prob = jax.nn.softmax((t @ w_grp + b_grp).astype(jnp.float32), axis=-1)
    g_w, g_idx = lax.top_k(g_prob, 1)
    e_logits = (t @ w_exp_r + b_exp_r).astype(jnp.float32).reshape(T, N_GROUPS, EXPERTS_PER_GROUP)
    e_in = jnp.take_along_axis(e_logits, g_idx[:, :, None], axis=1)[:, 0]
    e_top, e_idx = lax.top_k(e_in, TOP_K)
    e_w = jax.nn.softmax(e_top, axis=-1) * g_w
    flat_idx = g_idx * EXPERTS_PER_GROUP + e_idx
    combine = jnp.einsum('tk,tke->te', e_w, jax.nn.one_hot(flat_idx, N_EXPERTS, dtype=jnp.float32))
    hg = jnp.einsum('td,edf->tef', t, w_gate)
    hu = jnp.einsum('td,edf->tef', t, w_up)
    act = jax.nn.silu(hg) * hu * combine[..., None].astype(t.dtype)
    return jnp.einsum('tef,efd->td', act, w_down)


def setup_inputs(seed: int = 0) -> dict:
    key = jax.random.key(seed)
    ks = jax.random.split(key, 26)
    f32 = jnp.float32
    nrm = lambda k, shape, s: jax.random.normal(k, shape, f32) * s
    L = DEPTH
    D = D_MODEL
    offsets = jax.random.randint(ks[2], (BATCH, 1), 0, 2048, dtype=jnp.int32)
    positions = offsets + jnp.arange(SEQ, dtype=jnp.int32)[None, :]
    return {
        "x": nrm(ks[0], (BATCH, SEQ, D), 1.0),
        "c": nrm(ks[1], (BATCH, D), 1.0),
        "positions": positions,
        "w_ada": nrm(ks[3], (L, D, 6 * D), 0.5 * D ** -0.5),
        "b_ada": nrm(ks[4], (L, 6 * D), 0.01),
        "norm_mix_g": 1.0 + nrm(ks[5], (L, D), 0.01),
        "w_in": nrm(ks[6], (L, D, IN_PROJ_WIDTH), D ** -0.5),
        "lambda_q1": nrm(ks[7], (L, HEAD_DIM), 0.1),
        "lambda_k1": nrm(ks[8], (L, HEAD_DIM), 0.1),
        "lambda_q2": nrm(ks[9], (L, HEAD_DIM), 0.1),
        "lambda_k2": nrm(ks[10], (L, HEAD_DIM), 0.1),
        "subln_g": 1.0 + nrm(ks[11], (L, V_DIM), 0.01),
        "conv_w": nrm(ks[12], (L, CONV_K, CONV_WIDTH), CONV_K ** -0.5),
        "w_up_att": nrm(ks[13], (L, ATT_V_WIDTH, D), ATT_V_WIDTH ** -0.5),
        "w_up_conv": nrm(ks[14], (L, CONV_WIDTH, D), CONV_WIDTH ** -0.5),
        "w_out": nrm(ks[15], (L, D, D), D ** -0.5),
        "norm_ffn_g": 1.0 + nrm(ks[16], (L, D), 0.01),
        "w_group_router": nrm(ks[17], (L, D, N_GROUPS), D ** -0.5),
        "b_group_router": nrm(ks[18], (L, N_GROUPS), 0.01),
        "w_expert_router": nrm(ks[19], (L, D, N_EXPERTS), D ** -0.5),
        "b_expert_router": nrm(ks[20], (L, N_EXPERTS), 0.01),
        "w_gate": nrm(ks[21], (L, N_EXPERTS, D, EXPERT_FF), D ** -0.5),
        "w_up": nrm(ks[22], (L, N_EXPERTS, D, EXPERT_FF), D ** -0.5),
        "w_down": nrm(ks[23], (L, N_EXPERTS, EXPERT_FF, D), EXPERT_FF ** -0.5),
        "final_norm_g": 1.0 + nrm(ks[24], (D,), 0.01),
    }


def reference(x, c, positions, w_ada, b_ada, norm_mix_g, w_in, lambda_q1, lambda_k1,
              lambda_q2, lambda_k2, subln_g, conv_w, w_up_att, w_up_conv, w_out,
              norm_ffn_g, w_group_router, b_group_router, w_expert_router,
              b_expert_router, w_gate, w_up, w_down, final_norm_g):
    B, S, D = x.shape
    split_pts = [ATT_QK_WIDTH, 2 * ATT_QK_WIDTH, 3 * ATT_QK_WIDTH,
                 3 * ATT_QK_WIDTH + CONV_WIDTH, 3 * ATT_QK_WIDTH + 2 * CONV_WIDTH,
                 3 * ATT_QK_WIDTH + 3 * CONV_WIDTH]
    c_act = jax.nn.silu(c)
    for l in range(DEPTH):
        lambda_init = 0.8 - 0.6 * math.exp(-0.3 * l)
        mod = c_act @ w_ada[l] + b_ada[l]
        sh_m, sc_m, g_m, sh_f, sc_f, g_f = [m[:, None, :] for m in jnp.split(mod, 6, axis=-1)]

        h = rms_norm(x, norm_mix_g[l]) * (1.0 + sc_m) + sh_m
        z = h @ w_in[l]
        q, k, v, cb, cc, cu, gate_logits = jnp.split(z, split_pts, axis=-1)

        q = rope(q.reshape(B, S, N_HEADS, 2, HEAD_DIM), positions)
        k = rope(k.reshape(B, S, N_HEADS, 2, HEAD_DIM), positions)
        v = v.reshape(B, S, N_HEADS, V_DIM)
        lam = (jnp.exp(jnp.sum(lambda_q1[l].astype(jnp.float32) * lambda_k1[l].astype(jnp.float32)))
               - jnp.exp(jnp.sum(lambda_q2[l].astype(jnp.float32) * lambda_k2[l].astype(jnp.float32)))
               + lambda_init)
        att = diff_attention(q, k, v, lam)
        att = (rms_norm(att, subln_g[l]) * (1.0 - lambda_init)).reshape(B, S, ATT_V_WIDTH)

        conv = cb * causal_short_conv(cc * cu, conv_w[l])

        g_att, g_conv = jnp.split(gate_logits, N_BRANCHES, axis=-1)
        merged = jax.nn.sigmoid(g_att) * (att @ w_up_att[l]) + jax.nn.sigmoid(g_conv) * (conv @ w_up_conv[l])
        x = x + g_m * (merged @ w_out[l])

        h2 = rms_norm(x, norm_ffn_g[l]) * (1.0 + sc_f) + sh_f
        y = hierarchical_moe(h2.reshape(B * S, D), w_group_router[l], b_group_router[l],
                             w_expert_router[l], b_expert_router[l],
                             w_gate[l], w_up[l], w_down[l])
        x = x + g_f * y.reshape(B, S, D)
    return rms_norm(x, final_norm_g)
```

```python
import math
import os
from contextlib import ExitStack

import numpy as np
import concourse.bass as bass
import concourse.mybir as mybir
from concourse.bass_utils import run_bass_kernel_spmd


ENGS = ("pe", "act", "dve", "pool", "sp")


class _Op:
    __slots__ = ("eng", "fn", "deps", "is_dma", "semkey", "count", "sig", "signal",
                 "clock", "raw_deps", "idx", "no_ins")

    def __init__(self, eng, fn):
        self.eng = eng
        self.fn = fn
        self.deps = []
        self.raw_deps = set()
        self.is_dma = False
        self.semkey = None
        self.count = 0
        self.sig = 0
        self.signal = False
        self.clock = None
        self.no_ins = False


class Sched:
    _ninst = 0

    def __init__(self, nc):
        self.nc = nc
        self.tag = "k%d" % Sched._ninst
        Sched._ninst += 1
        self.ops = []
        self.last_writer = {}
        self.readers = {}
        self.dma_count = {}

    cut = None
    force = False

    def _add(self, o, reads, writes):
        if self.cut is not None and len(self.ops) >= self.cut and not self.force:
            return o
        deps = {}
        for k in reads:
            w = self.last_writer.get(k)
            if w is not None:
                deps[id(w)] = w
                o.raw_deps.add(id(w))
        for k in writes:
            w = self.last_writer.get(k)
            if w is not None:
                if not (o.is_dma and w.is_dma and o.semkey == w.semkey):
                    deps[id(w)] = w
            for r in self.readers.get(k, ()):
                if r is not o:
                    deps[id(r)] = r
        for k in reads:
            self.readers.setdefault(k, []).append(o)
        for k in writes:
            self.last_writer[k] = o
            self.readers[k] = []
        o.deps = list(deps.values())
        o.idx = len(self.ops)
        self.ops.append(o)
        return o

    def op(self, eng, fn, reads=(), writes=(), no_ins=False):
        o = _Op(eng, fn)
        o.no_ins = no_ins
        return self._add(o, reads, writes)

    def dma(self, queue, fn, reads=(), writes=(), semkey=None):
        o = _Op(queue, fn)
        o.is_dma = True
        o.semkey = semkey
        self.dma_count[semkey] = self.dma_count.get(semkey, 0) + 1
        o.count = self.dma_count[semkey]
        return self._add(o, reads, writes)

    def finalize(self, stack):
        nc = self.nc
        for o in self.ops:
            kept = []
            for d in o.deps:
                if d.is_dma:
                    kept.append(d)
                    continue
                if d.eng == o.eng and not o.is_dma:
                    if o.eng == "pe":
                        continue
                    if id(d) not in o.raw_deps:
                        continue
                kept.append(d)
                d.signal = True
            o.deps = kept
        cnt = {e: 0 for e in ENGS}
        for o in self.ops:
            if not o.is_dma and o.signal:
                cnt[o.eng] += 1
                o.sig = cnt[o.eng]
        self.sig_total = cnt
        esem = {e: stack.enter_context(nc.semaphore("s_%s_%s" % (self.tag, e))) for e in ENGS}
        dsem = {}
        for k in self.dma_count:
            dsem[k] = stack.enter_context(nc.semaphore("d_%s_%s" % (self.tag, str(k))))
        self.esem, self.dsem = esem, dsem
        known = {e: {} for e in ENGS}
        prog = {e: [] for e in ENGS}
        nwait = 0
        for o in self.ops:
            kn = known[o.eng]
            need = {}
            for d in o.deps:
                if d.is_dma:
                    name, val = ("d", d.semkey), 16 * d.count
                else:
                    name, val = ("e", d.eng), d.sig
                if kn.get(name, 0) >= val:
                    continue
                if need.get(name, 0) < val:
                    need[name] = val
            for d in o.deps:
                if d.clock:
                    for n2, v2 in d.clock.items():
                        if kn.get(n2, 0) < v2:
                            kn[n2] = v2
            for name, val in need.items():
                if kn.get(name, 0) < val:
                    kn[name] = val
            waits = []
            for name, val in need.items():
                sem = dsem[name[1]] if name[0] == "d" else esem[name[1]]
                waits.append((sem, val))
                nwait += 1
            if o.is_dma:
                o.clock = dict(kn)
                o.clock[("d", o.semkey)] = max(o.clock.get(("d", o.semkey), 0), 0)
            elif o.signal:
                o.clock = dict(kn)
                o.clock[("e", o.eng)] = o.sig
            prog[o.eng].append((waits, o))
        self.prog = prog
        self.nwait = nwait
        return prog

    def emit(self, block):
        nc = self.nc
        esem, dsem = self.esem, self.dsem

        def run(e, handle):
            for waits, o in self.prog[e]:
                for sem, val in waits:
                    handle.wait_ge(sem, val)
                ins = o.fn(handle)
                if ins is None:
                    continue
                if o.is_dma:
                    ins.then_inc(dsem[o.semkey], 16)
                elif o.signal:
                    ins.then_inc(esem[e], 1)

        self._run = run
        if block is None:
            return

        @block.tensor
        def _(h):
            run("pe", h)

        @block.scalar
        def _(h):
            run("act", h)

        @block.vector
        def _(h):
            run("dve", h)

        @block.gpsimd
        def _(h):
            run("pool", h)

        @block.sync
        def _(h):
            run("sp", h)

    def barrier(self):
        last = {}
        for o in self.ops:
            if getattr(o, "no_ins", False):
                continue
            if o.is_dma:
                last[("d", o.semkey)] = o
            else:
                last[("e", o.eng)] = o
        deps = list(last.values())
        for e in ENGS:
            b = _Op(e, lambda h: None)
            b.no_ins = True
            b.deps = list(deps)
            b.raw_deps = set(id(d) for d in deps)
            b.idx = len(self.ops)
            self.ops.append(b)


F32 = mybir.dt.float32
BF16 = mybir.dt.bfloat16
I32 = mybir.dt.int32
AF = mybir.ActivationFunctionType
ALU = mybir.AluOpType
AX = mybir.AxisListType

D = 1024
SEQ = 4096
NB = 4
TOWN = 2048
NE = 32
FF = 256
EPS = 1e-6
LAMBDA_INIT = 0.8 - 0.6 * math.exp(0.0)
TWO_PI = 2.0 * math.pi
C1 = 6.28125
C2 = TWO_PI - C1
OWN_CHUNKS = ([0, 3, 4, 7], [1, 2, 5, 6])
ARENA_BYTES = 206 * 1024
CAP = 512
HCAP = 256


class Arena:
    def __init__(self, ap):
        self.ap = ap
        self.top = 0
        self.limit = ap.shape[1] * 2

    def mark(self):
        return self.top

    def release(self, m):
        self.top = m

    def alloc(self, shape, dt):
        sz = 4 if dt in (F32, I32) else 2
        n = 1
        for s in shape:
            n *= s
        nbytes = (n * sz + 63) // 64 * 64
        off = self.top
        self.top += nbytes
        assert self.top <= self.limit, ("SBUF arena overflow", self.top, self.limit)
        v = self.ap[:, off // 2: off // 2 + (n * sz) // 2]
        if sz == 4:
            v = v.bitcast(dt)
        if len(shape) == 2:
            return v.rearrange("p (a b) -> p a b", a=shape[0])
        if len(shape) == 3:
            return v.rearrange("p (a b c) -> p a b c", a=shape[0], b=shape[1])
        return v


def build_program(stop_after=99, dumps=()):
    nc = bass.Bass("TRN2", target_bir_lowering=False)
    dt_in = {}

    def din(name, shape, dt=F32):
        dt_in[name] = nc.dram_tensor(name, list(shape), dt, kind="ExternalInput").ap()
        return dt_in[name]

    xo = din("xo", [TOWN, D])
    xs = din("xs", [SEQ, D])
    xh = din("xh", [8, D])
    hv = din("hv", [1, 8])
    cT = din("cT", [128, 8])
    pos_s = din("pos_s", [1, SEQ], I32)
    pos_o = din("pos_o", [1, TOWN], I32)
    qidx = din("qidx", [1, TOWN])
    kidx = din("kidx", [128, 32])
    invf = din("invf", [128, 1])
    sgn = din("sgn", [128, 1])
    w_ada = din("w_ada", [D, 6 * D])
    b_fm = din("b_fm", [128, 32])
    b_ada = din("b_ada", [1, 6 * D])
    gmix = din("gmix", [128, 8])
    gffn = din("gffn", [128, 8])
    gfin = din("gfin", [1, D])
    winx = din("winx", [D, 6144])
    lamv = din("lamv", [1, 256])
    subg = din("subg", [1, 128])
    subgc = din("subgc", [128, 1])
    cwT = din("cwT", [128, 12])
    wua = din("wua", [512, D])
    wuc = din("wuc", [512, D])
    wout = din("wout", [D, D])
    wrt = din("wrt", [D, 36])
    brt = din("brt", [1, 36])
    wg = din("wg", [NE, D, FF])
    wu = din("wu", [NE, D, FF])
    wd = din("wd", [NE, FF, D])
    erow_d = din("erow", [1, 32])
    ut_d = din("ut", [128, 128])
    out = nc.dram_tensor("out", [TOWN, D], F32, kind="ExternalOutput").ap()
    sgd = nc.dram_tensor("sgd", [4, 4, 128, 2048], BF16).ap()
    HS = nc.dram_tensor("hs_scratch", [NE * CAP, D], BF16).ap()
    YS = nc.dram_tensor("ys_scratch", [NE * CAP, D], F32).ap()
    dump_out = {}

    st = ExitStack()
    with st:
        arena_t = st.enter_context(nc.sbuf_tensor("arena", [128, ARENA_BYTES // 2], BF16))
        A = Arena(arena_t[:, :])
        banks = [st.enter_context(nc.psum_tensor("bank%d" % i, [128, 512], F32)) for i in range(8)]

        def bk(i):
            return banks[i][:, :]

        def bkb(i):
            return banks[i][:, :].bitcast(BF16)

        subscheds = []
        S = Sched(nc)
        if os.environ.get("MK_CUT"):
            S.cut = int(os.environ["MK_CUT"])
        uid = [0]

        def K(name):
            uid[0] += 1
            return (name, uid[0])

        KT = A.alloc([4, 4, 1024], BF16)
        VA = A.alloc([32, 4, 130], BF16)
        ident_f = A.alloc([128], F32)
        ident_b = A.alloc([128], BF16)
        cact = A.alloc([8], F32)
        cTs = A.alloc([8], F32)
        MOD = A.alloc([32], F32)
        bfm = A.alloc([32], F32)
        G1 = A.alloc([8], F32)
        G2 = A.alloc([8], F32)
        gmix_s = A.alloc([8], F32)
        gffn_s = A.alloc([8], F32)
        invf_s = A.alloc([1], F32)
        sgn_s = A.alloc([1], F32)
        kidx_s = A.alloc([32], F32)
        cw_s = A.alloc([12], F32)
        eps_c = A.alloc([1], F32)
        zero_c = A.alloc([1], F32)
        lam_s = A.alloc([4], F32)
        SUBG = A.alloc([128], F32)
        subgc_s = A.alloc([1], F32)
        GMrep = A.alloc([1024], BF16)
        GFrep = A.alloc([1024], BF16)
        stat = A.alloc([160], F32)
        rstd = A.alloc([160], F32)
        lnv = A.alloc([160], F32)
        hv_s = A.alloc([8], F32)
        sqj = A.alloc([1024], BF16)
        XT_N = 2
        xt = [A.alloc([1024], F32) for _ in range(XT_N)]
        xn = [A.alloc([1024], BF16) for _ in range(2)]
        persist_mark = A.mark()

        def X1v(T):
            j, t = T // 4, T % 4
            if t < 2:
                base = KT[:, j].rearrange("p h k -> p (h k)")[:, t * 2048:(t + 1) * 2048]
            else:
                base = VA[:, 8 * j:8 * j + 8].rearrange("p a h k -> p (a h k)")[:, (t - 2) * 2048:(t - 1) * 2048]
            return base.bitcast(F32)

        def X1key(T):
            j, t = T // 4, T % 4
            return ("KT", j) if t < 2 else ("VA", j)

        def ld(dst, src, key, q="sp"):
            S.dma(q, lambda e, d=dst, s=src: e.dma_start(out=d, in_=s), writes=[key], semkey=key)

        S.op("pool", lambda e: e.memset(ident_f, 0.0), writes=["ident_f"])
        S.op("pool", lambda e: e.affine_select(out=ident_f, in_=ident_f, pattern=[[-1, 128]],
                                               compare_op=ALU.not_equal, fill=1.0, base=0, channel_multiplier=1),
             reads=["ident_f"], writes=["ident_f"])
        S.op("dve", lambda e: e.tensor_copy(out=ident_b, in_=ident_f), reads=["ident_f"], writes=["ident_b"])
        S.op("dve", lambda e: e.memset(eps_c, EPS), writes=["eps_c"])
        S.op("dve", lambda e: e.memset(zero_c, 0.0), writes=["zero_c"])
        S.op("dve", lambda e: e.memset(stat, 0.0), writes=[("stat", c_) for c_ in range(160)])
        S.op("pool", lambda e: e.memset(VA[:, :, :, 128:130], 1.0), writes=[("VA", j) for j in range(4)])
        ld(cTs, cT, "cTs")
        ld(bfm, b_fm, "bfm")
        ld(gmix_s, gmix, "gmix_s")
        ld(gffn_s, gffn, "gffn_s")
        ld(invf_s, invf, "invf_s")
        ld(sgn_s, sgn, "sgn_s")
        ld(kidx_s, kidx, "kidx_s")
        ld(cw_s, cwT, "cw_s")
        ld(hv_s, hv.partition_broadcast(128), "hv_s")
        ld(SUBG, subg.partition_broadcast(128), "SUBG")
        ld(subgc_s, subgc, "subgc_s")

        mA = A.mark()
        wa = [A.alloc([8, 512], F32) for _ in range(2)]
        crep = A.alloc([8, 128], F32)
        brow = A.alloc([512], F32)
        lamr = A.alloc([256], F32)
        lamp = A.alloc([128], F32)
        S.op("act", lambda e: e.activation(out=cact, in_=cTs, func=AF.Silu), reads=["cTs"], writes=["cact"])
        for kc in range(8):
            S.op("dve", lambda e, kc=kc: e.tensor_copy(out=crep[:, kc, :], in_=cact[:, kc:kc + 1].to_broadcast([128, 128])),
                 reads=["cact"], writes=["crep"])
        ld(lamr, lamv.partition_broadcast(128), "lamr")
        S.op("dve", lambda e: e.tensor_tensor(out=lamp.rearrange("p (a b) -> p a b", a=2),
                                              in0=lamr.rearrange("p (a b c) -> p a b c", a=2, b=2)[:, :, 0, :],
                                              in1=lamr.rearrange("p (a b c) -> p a b c", a=2, b=2)[:, :, 1, :], op=ALU.mult),
             reads=["lamr"], writes=["lamp"])
        S.op("dve", lambda e: e.reduce_sum(out=lam_s[:, 2:4], in_=lamp.rearrange("p (a b) -> p a b", a=2), axis=AX.X),
             reads=["lamp"], writes=["lam23"])
        S.op("act", lambda e: e.activation(out=lam_s[:, 2:4], in_=lam_s[:, 2:4], func=AF.Exp, bias=zero_c, scale=1.0),
             reads=["lam23", "zero_c"], writes=["lam23"])
        S.op("dve", lambda e: e.tensor_tensor(out=lam_s[:, 0:1], in0=lam_s[:, 2:3], in1=lam_s[:, 3:4], op=ALU.subtract),
             reads=["lam23"], writes=["lam0"])
        S.op("dve", lambda e: e.tensor_scalar(out=lam_s[:, 1:2], in0=lam_s[:, 0:1], scalar1=LAMBDA_INIT, scalar2=-1.0, op0=ALU.add, op1=ALU.mult),
             reads=["lam0"], writes=["nlam"])
        S.op("pool", lambda e: e.tensor_scalar(out=SUBG, in0=SUBG, scalar1=1.0 - LAMBDA_INIT, scalar2=None, op0=ALU.mult),
             reads=["SUBG"], writes=["SUBG"])
        S.op("pool", lambda e: e.tensor_scalar(out=subgc_s, in0=subgc_s, scalar1=1.0 - LAMBDA_INIT, scalar2=None, op0=ALU.mult),
             reads=["subgc_s"], writes=["subgc_s"])

        wada_v = w_ada.rearrange("(kc p) c -> p kc c", p=128)
        ct_order = [0, 1, 2, 3, 6, 7, 8, 9, 4, 5, 10, 11]
        fm_col = {0: 0, 1: 4, 2: 8, 3: 12, 6: 16, 7: 20, 8: 24, 9: 28}
        pm = bk(0)
        first_fm = [True]
        def adaln_tile(i, pbank):
            ct = ct_order[i]
            slot = i % 2
            S.dma("sp", lambda e: e.dma_start(out=wa[slot], in_=wada_v[:, :, ct * 512:(ct + 1) * 512]),
                  writes=[("wa", slot)], semkey=("wa", slot))
            if ct in fm_col:
                c0 = fm_col[ct]
                for jj in range(4):
                    for kc in range(8):
                        S.op("pe", lambda e, jj=jj, kc=kc: e.matmul(
                            bk(pbank)[:, jj:jj + 1], lhsT=wa[slot][:, kc, jj * 128:(jj + 1) * 128], rhs=cact[:, kc:kc + 1],
                            start=(kc == 0), stop=(kc == 7)),
                            reads=[("wa", slot), "cact"], writes=[("bank", pbank)])
                mkey = "MOD1" if c0 < 16 else "MOD2"
                S.op("dve", lambda e: e.tensor_tensor(out=MOD[:, c0:c0 + 4], in0=bk(pbank)[:, 0:4], in1=bfm[:, c0:c0 + 4], op=ALU.add),
                     reads=[("bank", pbank), "bfm"], writes=[mkey, ("MODp", ct)])
            else:
                half = 0 if ct in (4, 10) else 1
                dst = GMrep if ct in (4, 5) else GFrep
                for kc in range(8):
                    S.op("pe", lambda e, kc=kc: e.matmul(
                        bk(pbank), lhsT=crep[:, kc, :], rhs=wa[slot][:, kc, :], start=(kc == 0), stop=(kc == 7)),
                        reads=[("wa", slot), "crep"], writes=[("bank", pbank)])
                S.dma("sp", lambda e: e.dma_start(out=brow, in_=b_ada[:, ct * 512:(ct + 1) * 512].partition_broadcast(128)),
                      writes=["brow"], semkey="brow")
                S.op("dve", lambda e: e.tensor_tensor(
                    out=dst[:, half * 512:(half + 1) * 512], in0=bk(pbank), in1=brow, op=ALU.add),
                    reads=[("bank", pbank), "brow"], writes=[("Grep", id(dst), half)])

        for i in range(4):
            adaln_tile(i, i % 2)
        S.op("dve", lambda e: e.scalar_tensor_tensor(out=G1, in0=MOD[:, 8:16], scalar=1.0, in1=gmix_s, op0=ALU.add, op1=ALU.mult),
             reads=["MOD1", "gmix_s"], writes=["G1"])
        SH1 = MOD[:, 0:8]
        SH2 = MOD[:, 16:24]

        if "mod" in dumps:
            dump_out["mod"] = (MOD, [128, 32], F32, ["MOD1", "MOD2"])
            dump_out["g1"] = (G1, [128, 8], F32, ["G1"])
            dump_out["lam"] = (lam_s, [128, 4], F32, ["lam0", "nlam"])
            dump_out["gmrep"] = (GMrep, [128, 1024], BF16, [("Grep", id(GMrep), 0), ("Grep", id(GMrep), 1)])

        statc = [0]
        xtc = [0]
        xnc = [0]
        ptrc = [0]

        def rstd_col(src_ap, npart, src_keys, n_feat):
            c = statc[0]
            statc[0] += 1
            assert c < 160
            kk = ("stat", c)
            S.op("act", lambda e: e.activation(out=sqj[0:npart, 0:src_ap.shape[1]], in_=src_ap, func=AF.Square,
                                               accum_out=stat[0:npart, c:c + 1]),
                 reads=src_keys, writes=[kk, "sqj"])
            S.op("act", lambda e: e.activation(out=lnv[0:npart, c:c + 1], in_=stat[0:npart, c:c + 1], func=AF.Ln,
                                               bias=eps_c[0:npart, :], scale=1.0 / n_feat),
                 reads=[kk, "eps_c"], writes=[("ln", c)])
            S.op("act", lambda e: e.activation(out=rstd[0:npart, c:c + 1], in_=lnv[0:npart, c:c + 1], func=AF.Exp,
                                               bias=zero_c[0:npart, :], scale=-0.5),
                 reads=[("ln", c), "zero_c"], writes=[("rstd", c)])
            return rstd[0:npart, c:c + 1], ("rstd", c)

        evc = [0]

        def norm_front(src_dram, npart):
            slot = xtc[0] % XT_N
            xtc[0] += 1
            xk = ("xt", slot)
            S.dma("sp", lambda e: e.dma_start(out=xt[slot][0:npart, :], in_=src_dram), writes=[xk], semkey=xk)
            rc, rk = rstd_col(xt[slot][0:npart, :], npart, [xk], D)
            ns = xnc[0] % 2
            xnc[0] += 1
            nk = ("xn", ns)
            S.op("dve", lambda e: e.tensor_scalar(out=xn[ns][0:npart, :], in0=xt[slot][0:npart, :], scalar1=rc, scalar2=None, op0=ALU.mult),
                 reads=[xk, rk], writes=[nk])
            return (ns, nk, npart)

        def norm_back(fr, dst_kc, dst_rng, dst_key, Gs, SHs, gkeys, ptr_banks, evt):
            ns, nk, npart = fr
            pb = ptr_banks[ptrc[0] % len(ptr_banks)]
            ptrc[0] += 1
            pk_ = ("bank", pb)
            pv = bkb(pb).rearrange("p (a b) -> p a b", a=8)
            for kc in range(8):
                S.op("pe", lambda e, kc=kc: e.transpose(out=pv[:, kc, 0:npart], in_=xn[ns][0:npart, kc * 128:(kc + 1) * 128],
                                                        identity=ident_b[0:npart, 0:npart]),
                     reads=[nk, "ident_b"], writes=[pk_])
            for kc in range(8):
                S.op("act", lambda e, kc=kc: e.activation(out=dst_kc(kc), in_=pv[:, kc, 0:npart], func=AF.Identity,
                                                          scale=Gs[:, kc:kc + 1], bias=SHs[:, kc:kc + 1]),
                     reads=[pk_] + gkeys, writes=[dst_key])

        def trig_tables(pos_dram, n0, tabs, tkey, tmp):
            posi, ang, u, nf = tmp
            ni = posi
            r = u
            S.dma("sp", lambda e: e.dma_start(out=posi, in_=pos_dram[:, n0:n0 + 512].partition_broadcast(128)),
                  writes=["posi"], semkey="posi")
            S.op("dve", lambda e: e.tensor_scalar(out=ang, in0=posi, scalar1=invf_s, scalar2=None, op0=ALU.mult),
                 reads=["posi", "invf_s"], writes=["ang"])
            for which in range(2):
                add = 0.0 if which == 0 else math.pi / 2
                S.op("dve", lambda e, add=add: e.tensor_scalar(out=u, in0=ang, scalar1=add, scalar2=1.0 / TWO_PI, op0=ALU.add, op1=ALU.mult),
                     reads=["ang"], writes=["u"])
                S.op("dve", lambda e: e.tensor_copy(out=ni, in_=u), reads=["u"], writes=["posi"])
                S.op("dve", lambda e: e.tensor_copy(out=nf, in_=ni), reads=["posi"], writes=["nf"])
                S.op("dve", lambda e, add=add: e.tensor_scalar(out=r, in0=ang, scalar1=add, scalar2=None, op0=ALU.add),
                     reads=["ang"], writes=["u"])
                S.op("dve", lambda e: e.scalar_tensor_tensor(out=r, in0=nf, scalar=-C1, in1=r, op0=ALU.mult, op1=ALU.add),
                     reads=["nf", "u"], writes=["u"])
                S.op("dve", lambda e: e.scalar_tensor_tensor(out=r, in0=nf, scalar=-C2, in1=r, op0=ALU.mult, op1=ALU.add),
                     reads=["nf", "u"], writes=["u"])
                S.op("dve", lambda e: e.tensor_scalar(out=r, in0=r, scalar1=math.pi, scalar2=-math.pi, op0=ALU.min, op1=ALU.max),
                     reads=["u"], writes=["u"])
                if which == 0:
                    S.op("act", lambda e: e.activation(out=tabs[:, 1, :], in_=r, func=AF.Sin, scale=sgn_s, bias=zero_c),
                         reads=["u", "sgn_s", "zero_c"], writes=[tkey])
                else:
                    S.op("act", lambda e: e.activation(out=tabs[:, 0, :], in_=r, func=AF.Sin, scale=1.0, bias=zero_c),
                         reads=["u", "zero_c"], writes=[tkey])

        def wload(dst, c0, c1, key):
            S.dma("pool", lambda e: e.dma_start(out=dst, in_=winx.rearrange("(kc p) c -> p kc c", p=128)[:, :, c0:c1]),
                  writes=[key], semkey=key)

        def rope_proj(Wn, Wr, wkeys, hT_ap, hkeys, h, tabs, tkey, dst, dkey, pbanks, tmps, tmpkeys):
            pa, pr = pbanks
            for kc in range(8):
                S.op("pe", lambda e, kc=kc: e.matmul(bk(pa), lhsT=Wn[:, kc, h * 128:(h + 1) * 128], rhs=hT_ap[:, kc, :],
                                                     start=(kc == 0), stop=(kc == 7)),
                     reads=wkeys + hkeys, writes=[("bank", pa)])
            for kc in range(8):
                S.op("pe", lambda e, kc=kc: e.matmul(bk(pr), lhsT=Wr[:, kc, h * 128:(h + 1) * 128], rhs=hT_ap[:, kc, :],
                                                     start=(kc == 0), stop=(kc == 7)),
                     reads=wkeys + hkeys, writes=[("bank", pr)])
            ta, tb = tmps
            ka, kb = tmpkeys
            S.op("dve", lambda e: e.tensor_tensor(out=ta, in0=bk(pa), in1=tabs[:, 0, :], op=ALU.mult),
                 reads=[("bank", pa), tkey], writes=[ka])
            S.op("dve", lambda e: e.tensor_tensor(out=tb, in0=bk(pr), in1=tabs[:, 1, :], op=ALU.mult),
                 reads=[("bank", pr), tkey], writes=[kb])
            S.op("pool", lambda e: e.tensor_tensor(out=dst, in0=ta, in1=tb, op=ALU.add), reads=[ka, kb], writes=[dkey])

        def do_dumps_and_finish():
            S.force = True
            names = []
            for name, (ap, shape, dt, keys) in dump_out.items():
                if name not in dumps:
                    continue
                dd = nc.dram_tensor("dbg_" + name, list(shape), dt, kind="ExternalOutput").ap()
                S.dma("sp", lambda e, dd=dd, ap=ap: e.dma_start(out=dd, in_=ap), reads=list(keys), writes=["dbg_out"], semkey="dbg_out")
                names.append(name)
            S.op("sp", lambda e: None, reads=["dbg_out"] + [("out", T_) for T_ in range(16)])
            S.finalize(st)
            for sub in subscheds:
                sub.finalize(st)
                sub.emit(None)
            if os.environ.get("MK_VERBOSE"):
                print("ops", len(S.ops), "sig", S.sig_total, "waits", S.nwait, "sbuf top", A.top)
            with nc.Block() as block:
                S.emit(block)
            return nc

        if stop_after <= 0:
            S.barrier()
            return do_dumps_and_finish()

        m2 = A.mark()
        hTa = [A.alloc([8, 512], BF16) for _ in range(2)]
        Wk = A.alloc([8, 512], BF16)
        Wkr = A.alloc([8, 512], BF16)
        Wv = A.alloc([8, 512], BF16)
        tabs2 = [A.alloc([2, 512], F32) for _ in range(2)]
        trig_tmp = (A.alloc([512], I32), A.alloc([512], F32), A.alloc([512], F32), A.alloc([512], F32))
        tmpa = [A.alloc([512], F32) for _ in range(2)]
        tmpb = [A.alloc([512], F32) for _ in range(2)]
        wload(Wk, 512, 1024, "Wk")
        wload(Wkr, 5632, 6144, "Wkr")
        wload(Wv, 1024, 1536, "Wv")
        evt = [A.alloc([4, 128], F32) for _ in range(2)]
        rc = [0]
        fr_state = [norm_front(xs[0:128, :], 128)]

        def p2_tile(tb, t):
            T = tb * 4 + t
            hs = tb % 2
            fr = fr_state[0]
            if T + 1 < 32:
                fr_state[0] = norm_front(xs[(T + 1) * 128:(T + 2) * 128, :], 128)
            norm_back(fr, lambda kc, hs=hs, t=t: hTa[hs][:, kc, t * 128:(t + 1) * 128],
                      lambda k0, k1, hs=hs, t=t: hTa[hs][:, k0:k1, t * 128:(t + 1) * 128],
                      ("hTa", hs), G1, SH1, ["G1", "MOD1"], [0, 1], evt)

        trig_tables(pos_s, 0, tabs2[0], ("tabs2", 0), trig_tmp)
        for t in range(4):
            p2_tile(0, t)
        for tb in range(8):
            hs = tb % 2
            hk = ("hTa", hs)
            tk = ("tabs2", hs)
            adaln_tile(4 + tb, 6)
            if tb == 7:
                S.op("dve", lambda e: e.scalar_tensor_tensor(out=G2, in0=MOD[:, 24:32], scalar=1.0, in1=gffn_s, op0=ALU.add, op1=ALU.mult),
                     reads=["MOD2", "gffn_s"], writes=["G2"])
            if tb + 1 < 8:
                trig_tables(pos_s, (tb + 1) * 512, tabs2[1 - hs], ("tabs2", 1 - hs), trig_tmp)
            jb, off = tb // 2, (tb % 2) * 512
            for h in range(4):
                i = rc[0] % 2
                rc[0] += 1
                rope_proj(Wk, Wkr, ["Wk", "Wkr"], hTa[hs], [hk], h, tabs2[hs], tk,
                          KT[:, jb, h, off:off + 512], ("KT", jb), (2 + 2 * i, 3 + 2 * i),
                          (tmpa[i], tmpb[i]), (("tmpa", i), ("tmpb", i)))
                if tb + 1 < 8:
                    p2_tile(tb + 1, h)
            for t in range(4):
                kt = tb * 4 + t
                pb = 6 + (t % 2)
                for kc in range(8):
                    S.op("pe", lambda e, kc=kc, t=t, pb=pb, hs=hs: e.matmul(
                        bk(pb), lhsT=hTa[hs][:, kc, t * 128:(t + 1) * 128], rhs=Wv[:, kc, :], start=(kc == 0), stop=(kc == 7)),
                        reads=[hk, "Wv"], writes=[("bank", pb)])
                S.op("act", lambda e, kt=kt, pb=pb: e.activation(
                    out=VA[:, kt, :, 0:128], in_=bk(pb).rearrange("p (h v) -> p h v", h=4), func=AF.Copy),
                    reads=[("bank", pb)], writes=[("VA", kt // 8)])
        if "kt" in dumps:
            dump_out["kt"] = (KT.rearrange("p a h k -> p (a h k)"), [128, 16384], BF16, [("KT", j) for j in range(4)])
            dump_out["va"] = (VA.rearrange("p a h k -> p (a h k)"), [128, 32 * 4 * 130], BF16, [("VA", j) for j in range(4)])
        if stop_after <= 2:
            S.barrier()
            return do_dumps_and_finish()

        S.barrier()
        A.release(mA)
        Qs = A.alloc([4, TOWN], BF16)
        convT = A.alloc([4, TOWN], BF16)
        m3 = A.mark()
        hTo = A.alloc([4, 8, 512], BF16)
        hTh = A.alloc([8, 8], BF16)
        wr = [A.alloc([8, 512], BF16) for _ in range(3)]
        m3s = A.mark()
        Uh = A.alloc([4, 8], F32)
        Ub = [A.alloc([514], F32) for _ in range(2)]
        yb = [A.alloc([512], F32) for _ in range(2)]
        ccS = [A.alloc([512], F32) for _ in range(2)]
        evt = [A.alloc([4, 128], F32) for _ in range(2)]
        fr_next = norm_front(xo[0:128, :], 128)
        for T in range(16):
            j, t = T // 4, T % 4
            fr = fr_next
            fr_next = norm_front(xo[(T + 1) * 128:(T + 2) * 128, :], 128) if T + 1 < 16 else norm_front(xh[:, :], 8)
            norm_back(fr, lambda kc, j=j, t=t: hTo[:, j, kc, t * 128:(t + 1) * 128],
                      lambda k0, k1, j=j, t=t: hTo[:, j, k0:k1, t * 128:(t + 1) * 128],
                      ("hTo", j), G1, SH1, ["G1", "MOD1"], [0, 1], evt)
        norm_back(fr_next, lambda kc: hTh[:, kc, :], lambda k0, k1: hTh[:, k0:k1, :], "hTh", G1, SH1, ["G1", "MOD1"], [0, 1], evt)
        wload(wr[0], 1536, 2048, ("wr", 0))
        wload(wr[1], 2048, 2560, ("wr", 1))
        wload(wr[2], 2560, 3072, ("wr", 2))
        for cch in range(4):
            for kc in range(8):
                S.op("pe", lambda e, kc=kc, cch=cch: e.matmul(bk(2)[:, 0:8], lhsT=wr[1][:, kc, cch * 128:(cch + 1) * 128], rhs=hTh[:, kc, :],
                                                              start=(kc == 0), stop=(kc == 7)),
                     reads=[("wr", 1), "hTh"], writes=[("bank", 2)])
            for kc in range(8):
                S.op("pe", lambda e, kc=kc, cch=cch: e.matmul(bk(3)[:, 0:8], lhsT=wr[2][:, kc, cch * 128:(cch + 1) * 128], rhs=hTh[:, kc, :],
                                                              start=(kc == 0), stop=(kc == 7)),
                     reads=[("wr", 2), "hTh"], writes=[("bank", 3)])
            S.op("act", lambda e, cch=cch: e.activation(out=Uh[:, cch, :], in_=bk(2)[:, 0:8], func=AF.Copy),
                 reads=[("bank", 2)], writes=[("Uh", cch)])
            S.op("dve", lambda e, cch=cch: e.tensor_tensor(out=Uh[:, cch, :], in0=bk(3)[:, 0:8], in1=Uh[:, cch, :], op=ALU.mult),
                 reads=[("bank", 3), ("Uh", cch)], writes=[("Uh", cch)])
            S.op("dve", lambda e, cch=cch: e.tensor_tensor(out=Uh[:, cch, :], in0=Uh[:, cch, :], in1=hv_s, op=ALU.mult),
                 reads=[("Uh", cch), "hv_s"], writes=[("Uh", cch)])
        cnt = 0
        for j in range(4):
            for cch in range(4):
                i = cnt % 2
                cnt += 1
                b_cc, b_cu, b_cb = 2 + 3 * i, 3 + 3 * i, 4 + 3 * i
                for (wi, pb) in ((1, b_cc), (2, b_cu), (0, b_cb)):
                    for kc in range(8):
                        S.op("pe", lambda e, kc=kc, wi=wi, pb=pb, cch=cch, j=j: e.matmul(
                            bk(pb), lhsT=wr[wi][:, kc, cch * 128:(cch + 1) * 128], rhs=hTo[:, j, kc, :], start=(kc == 0), stop=(kc == 7)),
                            reads=[("wr", wi), ("hTo", j)], writes=[("bank", pb)])
                S.op("act", lambda e, i=i, b_cc=b_cc: e.activation(out=ccS[i], in_=bk(b_cc), func=AF.Copy),
                     reads=[("bank", b_cc)], writes=[("ccS", i)])
                S.op("dve", lambda e, i=i, b_cu=b_cu: e.tensor_tensor(out=Ub[i][:, 2:514], in0=bk(b_cu), in1=ccS[i], op=ALU.mult),
                     reads=[("bank", b_cu), ("ccS", i)], writes=[("Ub", i)])
                S.op("pool", lambda e, i=i, cch=cch, j=j: e.tensor_copy(out=Ub[i][:, 0:2], in_=Uh[:, cch, 2 * j:2 * j + 2]),
                     reads=[("Uh", cch)], writes=[("Ubh", i)])
                S.op("act", lambda e, i=i, cch=cch: e.activation(out=yb[i], in_=Ub[i][:, 2:514], func=AF.Copy, scale=cw_s[:, cch * 3:cch * 3 + 1]),
                     reads=[("Ub", i), "cw_s"], writes=[("yb", i)])
                S.op("dve", lambda e, i=i, cch=cch: e.scalar_tensor_tensor(out=yb[i], in0=Ub[i][:, 1:513], scalar=cw_s[:, cch * 3 + 1:cch * 3 + 2],
                                                                          in1=yb[i], op0=ALU.mult, op1=ALU.add),
                     reads=[("Ub", i), ("Ubh", i), ("yb", i), "cw_s"], writes=[("yb", i)])
                S.op("dve", lambda e, i=i, cch=cch: e.scalar_tensor_tensor(out=yb[i], in0=Ub[i][:, 0:512], scalar=cw_s[:, cch * 3 + 2:cch * 3 + 3],
                                                                          in1=yb[i], op0=ALU.mult, op1=ALU.add),
                     reads=[("Ub", i), ("Ubh", i), ("yb", i), "cw_s"], writes=[("yb", i)])
                S.op("dve", lambda e, i=i, cch=cch, j=j, b_cb=b_cb: e.tensor_tensor(
                    out=convT[:, cch, j * 512:(j + 1) * 512], in0=bk(b_cb), in1=yb[i], op=ALU.mult),
                    reads=[("bank", b_cb), ("yb", i)], writes=["convT"])
        S.barrier()
        A.release(m3s)
        tabs3 = [A.alloc([2, 512], F32) for _ in range(2)]
        trig_tmp = (A.alloc([512], I32), A.alloc([512], F32), A.alloc([512], F32), A.alloc([512], F32))
        tmpa = [A.alloc([512], F32) for _ in range(2)]
        tmpb = [A.alloc([512], F32) for _ in range(2)]
        wload(wr[0], 0, 512, ("wr", 0))
        wload(wr[1], 5120, 5632, ("wr", 1))
        rc = [0]
        for j in range(4):
            ts = j % 2
            tk = ("tabs3", ts)
            trig_tables(pos_o, j * 512, tabs3[ts], tk, trig_tmp)
            for h in range(4):
                i = rc[0] % 2
                rc[0] += 1
                rope_proj(wr[0], wr[1], [("wr", 0), ("wr", 1)], hTo[:, j], [("hTo", j)], h, tabs3[ts], tk,
                          Qs[:, h, j * 512:(j + 1) * 512], "Qs", (2 + 2 * i, 3 + 2 * i),
                          (tmpa[i], tmpb[i]), (("tmpa", i), ("tmpb", i)))
        S.barrier()
        A.release(m3s)
        sgb = [A.alloc([4, 512], BF16) for _ in range(2)]
        gcnt = 0
        for g in range(4):
            ws = (2, 0, 1, 2)[g]
            wload(wr[ws], 3072 + g * 512, 3072 + (g + 1) * 512, ("wr", ws))
            for j in range(4):
                si = gcnt % 2
                gcnt += 1
                for dq in range(4):
                    pb = 2 + (dq % 4)
                    for kc in range(8):
                        S.op("pe", lambda e, kc=kc, ws=ws, dq=dq, pb=pb, j=j: e.matmul(
                            bk(pb), lhsT=wr[ws][:, kc, dq * 128:(dq + 1) * 128], rhs=hTo[:, j, kc, :], start=(kc == 0), stop=(kc == 7)),
                            reads=[("wr", ws), ("hTo", j)], writes=[("bank", pb)])
                    S.op("act", lambda e, si=si, dq=dq, pb=pb: e.activation(out=sgb[si][:, dq, :], in_=bk(pb), func=AF.Sigmoid,
                                                                             bias=zero_c, scale=1.0),
                         reads=[("bank", pb), "zero_c"], writes=[("sgb", si)])
                S.dma("sp", lambda e, si=si, g=g, j=j: e.dma_start(out=sgd[g, j].rearrange("p (a b) -> p a b", a=4), in_=sgb[si]),
                      reads=[("sgb", si)], writes=[("sgd", g, j)], semkey=("sgd", si))
        if "q" in dumps:
            dump_out["q"] = (Qs.rearrange("p h t -> p (h t)"), [128, 4 * TOWN], BF16, ["Qs"])
            dump_out["conv"] = (convT.rearrange("p h t -> p (h t)"), [128, 4 * TOWN], BF16, ["convT"])
        if stop_after <= 3:
            S.barrier()
            return do_dumps_and_finish()

        S.barrier()
        A.release(m3)
        m4 = A.mark()
        Wua = A.alloc([4, 1024], BF16)
        Wuc = A.alloc([4, 1024], BF16)
        Wo = A.alloc([8, 1024], BF16)
        pT = [A.alloc([512], BF16) for _ in range(4)]
        Qp = [A.alloc([2, 2, 256], BF16) for _ in range(2)]
        qrep2 = A.alloc([2, 2, 256], F32)
        ones_b = A.alloc([128], BF16)
        rL = A.alloc([512], F32)
        tO = A.alloc([512], F32)
        oo = A.alloc([256], F32)
        o2b = A.alloc([256], BF16)
        ors = A.alloc([256], F32)
        qpc = 0
        qslot = {}
        S.op("pool", lambda e: e.memset(ones_b, 1.0), writes=["ones_b"])
        for qs_ in range(2):
            S.op("pool", lambda e, qs_=qs_: e.memset(Qp[qs_], 0.0), writes=[("Qpz", qs_), ("Qp", qs_)])
        attTs = [A.alloc([4, 512], BF16) for _ in range(2)]
        carry = []
        xslot = {}
        jcount = 0
        mergedT = A.alloc([8, 512], BF16)
        m1b = [A.alloc([512], F32) for _ in range(2)]
        m2b = [A.alloc([512], F32) for _ in range(2)]
        sgl = [A.alloc([2, 512], BF16) for _ in range(3)]
        qrep = A.alloc([512], F32)
        S.dma("pool", lambda e: e.dma_start(out=Wua, in_=wua.rearrange("(kc p) c -> p kc c", p=128)), writes=["Wua"], semkey="Wua")
        S.dma("pool", lambda e: e.dma_start(out=Wuc, in_=wuc.rearrange("(kc p) c -> p kc c", p=128)), writes=["Wuc"], semkey="Wuc")
        S.dma("pool", lambda e: e.dma_start(out=Wo, in_=wout.rearrange("(kc p) c -> p kc c", p=128)), writes=["Wo"], semkey="Wo")
        wo_scaled = [False]
        stc = 0
        ptc = 0
        sgc = 0
        mc = 0
        for j in (3, 2, 1, 0):
            nkt = 8 * (j + 1)
            S.dma("sp", lambda e, j=j: e.dma_start(out=qrep, in_=qidx[:, j * 512:(j + 1) * 512].partition_broadcast(128)),
                  writes=["qrep"], semkey="qrep")
            S.op("dve", lambda e: e.tensor_copy(out=qrep2, in_=qrep.rearrange("p (a b) -> p a b", a=2).unsqueeze(2).to_broadcast([128, 2, 2, 256])),
                 reads=["qrep"], writes=["qrep2"])
            nkq = (nkt - 2, nkt)
            items = [(h, qh, kt) for h in range(4) for qh in range(2) for kt in range(nkq[qh])]
            LOOK = 3
            slots = {}
            asl = jcount % 2
            jcount += 1
            pending = []
            for step in range(len(items) + LOOK):
                while pending and pending[0][0] <= step:
                    pending.pop(0)[1]()
                if carry and step >= 2 and step % 2 == 0:
                    carry.pop(0)()
                if step < len(items):
                    h, qh, kt = items[step]
                    if qh == 0 and kt == 0:
                        qs_ = qpc % 2
                        qpc += 1
                        qslot[h] = qs_
                        for m in range(2):
                            S.op("pool", lambda e, m=m, h=h, j=j, qs_=qs_: e.tensor_copy(
                                out=Qp[qs_][m * 64:(m + 1) * 64, :, m, :],
                                in_=Qs[m * 64:(m + 1) * 64, h, j * 512:(j + 1) * 512].rearrange("p (a b) -> p a b", a=2)),
                                reads=["Qs", ("Qpz", qs_)], writes=[("Qp", qs_)])
                    qs_ = qslot[h]
                    sb_ = stc % 3
                    stc += 1
                    pi = ptc % 4
                    ptc += 1
                    slots[step] = pi
                    jb, ko = kt // 8, (kt % 8) * 128
                    S.op("pe", lambda e, sb_=sb_, jb=jb, ko=ko, h=h, qh=qh, qs_=qs_: e.matmul(
                        bk(sb_), lhsT=KT[:, jb, h, ko:ko + 128], rhs=Qp[qs_][:, qh].rearrange("p m q -> p (m q)"),
                        start=True, stop=True),
                        reads=[("KT", jb), ("Qp", qs_)], writes=[("bank", sb_)])
                    S.op("act", lambda e, sb_=sb_, pi=pi: e.activation(out=pT[pi], in_=bk(sb_), func=AF.Exp, bias=zero_c, scale=0.125),
                         reads=[("bank", sb_), "zero_c"], writes=[("pT", pi)])
                    if kt >= 8 * j:
                        S.op("dve", lambda e, pi=pi, kt=kt, qh=qh: e.scalar_tensor_tensor(
                            out=pT[pi], in0=qrep2[:, qh].rearrange("p m q -> p (m q)"), scalar=kidx_s[:, kt:kt + 1], in1=pT[pi],
                            op0=ALU.is_ge, op1=ALU.mult),
                            reads=[("pT", pi), "qrep2", "kidx_s"], writes=[("pT", pi)])
                if step - LOOK < 0:
                    continue
                h, qh, kt = items[step - LOOK]
                pi = slots.pop(step - LOOK)
                ai = (h * 2 + qh) % 2
                bo, bl = 3 + 2 * ai, 4 + 2 * ai
                nkt_ = nkq[qh]
                S.op("pe", lambda e, pi=pi, kt=kt, h=h, nkt=nkt_, bo=bo: e.matmul(
                    bk(bo), lhsT=VA[:, kt, h, 0:128], rhs=pT[pi], start=(kt == 0), stop=(kt == nkt - 1)),
                    reads=[("pT", pi), ("VA", kt // 8)], writes=[("bank", bo)])
                S.op("pe", lambda e, pi=pi, kt=kt, nkt=nkt_, bl=bl: e.matmul(
                    bk(bl), lhsT=ones_b, rhs=pT[pi], start=(kt == 0), stop=(kt == nkt - 1)),
                    reads=[("pT", pi), "ones_b"], writes=[("bank", bl)])
                if kt != nkt_ - 1:
                    continue
                S.op("dve", lambda e, bl=bl: e.reciprocal(out=rL, in_=bk(bl)), reads=[("bank", bl)], writes=["rL"])
                S.op("dve", lambda e, bo=bo: e.tensor_tensor(out=tO, in0=bk(bo), in1=rL, op=ALU.mult), reads=[("bank", bo), "rL"], writes=["tO"])
                S.op("dve", lambda e: e.scalar_tensor_tensor(out=oo, in0=tO[:, 256:512], scalar=lam_s[:, 1:2], in1=tO[:, 0:256],
                                                             op0=ALU.mult, op1=ALU.add),
                     reads=["tO", "nlam"], writes=["oo"])
                S.op("pool", lambda e: e.tensor_tensor(out=o2b, in0=oo, in1=oo, op=ALU.mult), reads=["oo"], writes=["o2b"])

                def _subln_tail(h=h, qh=qh, asl_=asl):
                    S.op("pe", lambda e: e.matmul(bk(7)[:, 0:256], lhsT=ones_b, rhs=o2b, start=True, stop=True),
                         reads=["o2b", "ones_b"], writes=[("bank", 7)])
                    S.op("act", lambda e: e.activation(out=ors, in_=bk(7)[:, 0:256], func=AF.Ln, bias=eps_c, scale=1.0 / 128),
                         reads=[("bank", 7), "eps_c"], writes=["ors"])
                    S.op("act", lambda e: e.activation(out=ors, in_=ors, func=AF.Exp, bias=zero_c, scale=-0.5), reads=["ors", "zero_c"], writes=["ors"])
                    S.op("dve", lambda e: e.scalar_tensor_tensor(out=attTs[asl_][:, h, qh * 256:(qh + 1) * 256], in0=oo, scalar=subgc_s, in1=ors,
                                                                 op0=ALU.mult, op1=ALU.mult),
                         reads=["oo", "ors", "subgc_s"], writes=[("attT", asl_)])
                pending.append((step + 5, _subln_tail))
            for _, fn_ in pending:
                fn_()
            pending = []
            while carry:
                carry.pop(0)()
            if not wo_scaled[0]:
                wo_scaled[0] = True
                for kc in range(8):
                    S.op("dve", lambda e, kc=kc: e.tensor_tensor(out=Wo[:, kc, :], in0=Wo[:, kc, :], in1=GMrep, op=ALU.mult),
                         reads=["Wo", ("Grep", id(GMrep), 0), ("Grep", id(GMrep), 1)], writes=["Wo"])
            overl = (j != 0)
            chunks = []
            for dc in range(8):
                si = sgc % 3
                sgc += 1
                mi = mc % 2
                mc += 1
                g_a, g_c, dq = dc // 4, 2 + dc // 4, dc % 4
                ba = 7 if overl else 6
                bc_ = 7

                def _ca(dc=dc, si=si, mi=mi, g_a=g_a, g_c=g_c, dq=dq, j=j, ba=ba, asl=asl):
                    S.dma("sp", lambda e: e.dma_start(out=sgl[si][:, 0, :], in_=sgd[g_a, j, :, dq * 512:(dq + 1) * 512]),
                          reads=[("sgd", g_a, j)], writes=[("sgl", si)], semkey=("sgl", si))
                    S.dma("sp", lambda e: e.dma_start(out=sgl[si][:, 1, :], in_=sgd[g_c, j, :, dq * 512:(dq + 1) * 512]),
                          reads=[("sgd", g_c, j)], writes=[("sgl", si)], semkey=("sgl", si))
                    for kc in range(4):
                        S.op("pe", lambda e, kc=kc: e.matmul(bk(ba), lhsT=Wua[:, kc, dc * 128:(dc + 1) * 128], rhs=attTs[asl][:, kc, :],
                                                             start=(kc == 0), stop=(kc == 3)),
                             reads=["Wua", ("attT", asl)], writes=[("bank", ba)])
                    S.op("dve", lambda e: e.tensor_tensor(out=m1b[mi], in0=bk(ba), in1=sgl[si][:, 0, :], op=ALU.mult),
                         reads=[("bank", ba), ("sgl", si)], writes=[("m1b", mi)])

                def _cc(dc=dc, si=si, mi=mi, j=j, bc_=bc_):
                    for kc in range(4):
                        S.op("pe", lambda e, kc=kc: e.matmul(bk(bc_), lhsT=Wuc[:, kc, dc * 128:(dc + 1) * 128], rhs=convT[:, kc, j * 512:(j + 1) * 512],
                                                             start=(kc == 0), stop=(kc == 3)),
                             reads=["Wuc", "convT"], writes=[("bank", bc_)])
                    S.op("dve", lambda e: e.tensor_tensor(out=m2b[mi], in0=bk(bc_), in1=sgl[si][:, 1, :], op=ALU.mult),
                         reads=[("bank", bc_), ("sgl", si)], writes=[("m2b", mi)])
                    S.op("pool", lambda e: e.tensor_tensor(out=mergedT[:, dc, :], in0=m1b[mi], in1=m2b[mi], op=ALU.add),
                         reads=[("m1b", mi), ("m2b", mi)], writes=["mergedT"])
                chunks.append(_ca)
                chunks.append(_cc)
            for t in range(4):
                T = j * 4 + t
                for dh in range(2):
                    pb = 7 if overl else 6 + dh

                    def _co(t=t, T=T, dh=dh, pb=pb):
                        if dh == 0:
                            slot = xtc[0] % XT_N
                            xtc[0] += 1
                            xslot[T] = slot
                            S.dma("sp", lambda e: e.dma_start(out=xt[slot], in_=xo[T * 128:(T + 1) * 128, :]),
                                  writes=[("xt", slot)], semkey=("xt", slot))
                        slot = xslot[T]
                        xk = ("xt", slot)
                        for kc in range(8):
                            S.op("pe", lambda e, kc=kc: e.matmul(
                                bk(pb), lhsT=mergedT[:, kc, t * 128:(t + 1) * 128], rhs=Wo[:, kc, dh * 512:(dh + 1) * 512],
                                start=(kc == 0), stop=(kc == 7)),
                                reads=["mergedT", "Wo"], writes=[("bank", pb)])
                        S.op("dve", lambda e: e.tensor_tensor(
                            out=X1v(T)[:, dh * 512:(dh + 1) * 512], in0=bk(pb), in1=xt[slot][:, dh * 512:(dh + 1) * 512], op=ALU.add),
                            reads=[("bank", pb), xk], writes=[X1key(T), ("X1", T)])
                    chunks.append(_co)
            if overl:
                carry = chunks
            else:
                for c_ in chunks:
                    c_()
        if "x1" in dumps:
            for T in (0, 5, 15):
                dump_out["x1_%d" % T] = (X1v(T), [128, 1024], F32, [("X1", T)])
        if stop_after <= 4:
            S.barrier()
            return do_dumps_and_finish()

        S.barrier()
        A.release(persist_mark)
        m5 = A.mark()
        XNB = A.alloc([16, 1024], BF16)
        idx_i = A.alloc([16, 2], I32)
        wsel = A.alloc([16, 2], F32)
        ones_b5 = A.alloc([128], BF16)
        utb = A.alloc([128], BF16)
        erow = A.alloc([32], F32)
        comb = A.alloc([16, 32], F32)
        flag_f = A.alloc([2], F32)
        flag_i = A.alloc([2], I32)
        m5b = A.mark()
        xn32s = [A.alloc([1024], F32) for _ in range(2)]
        h2f = [A.alloc([8, 128], F32) for _ in range(2)]
        LG = A.alloc([16, 36], F32)
        wrt_s = A.alloc([8, 36], F32)
        brt_s = A.alloc([36], F32)
        utf = A.alloc([128], F32)
        r_gmax = A.alloc([16], F32)
        r_gl = A.alloc([16, 4], F32)
        r_gsum = A.alloc([16], F32)
        r_gw = A.alloc([16], F32)
        r_gmask = A.alloc([16, 4], F32)
        r_elm = A.alloc([16, 32], F32)
        r_m1 = A.alloc([16], F32)
        r_mask1 = A.alloc([16, 32], F32)
        r_elm2 = A.alloc([16, 32], F32)
        r_m2 = A.alloc([16], F32)
        r_mask2 = A.alloc([16, 32], F32)
        r_d = A.alloc([16], F32)
        r_w1 = A.alloc([16], F32)
        r_w2 = A.alloc([16], F32)
        r_tmp = A.alloc([16, 32], F32)
        r_e = A.alloc([16, 2], F32)
        r_p = A.alloc([16, 2], F32)
        r_ov = A.alloc([16, 2], F32)
        Mb = A.alloc([16, 32], BF16)
        posA = A.alloc([16, 32], F32)
        ld(wrt_s, wrt.rearrange("(kc p) c -> p kc c", p=128), "wrt_s")
        ld(brt_s, brt.partition_broadcast(128), "brt_s")
        ld(erow, erow_d.partition_broadcast(128), "erow")
        ld(utf, ut_d, "utf")
        S.op("dve", lambda e: e.tensor_copy(out=utb, in_=utf), reads=["utf"], writes=["utb"])
        S.op("pool", lambda e: e.memset(ones_b5, 1.0), writes=["ones_b5"])
        def r_front(T):
            rc_, rk = rstd_col(X1v(T), 128, [("X1", T)], D)
            xs_ = T % 2
            S.op("dve", lambda e: e.tensor_scalar(out=xn32s[xs_], in0=X1v(T), scalar1=rc_, scalar2=None, op0=ALU.mult),
                 reads=[("X1", T), rk], writes=[("xn32", xs_)])
            S.op("pool", lambda e: e.tensor_copy(out=XNB[:, T, :], in_=xn32s[xs_]), reads=[("xn32", xs_)], writes=[("XNB", T)])

        def r_back(T):
            xs_ = T % 2
            hi = T % 2
            b0 = 0 if T % 2 == 0 else 4
            for half in range(2):
                pb = b0 + half
                for q4 in range(4):
                    kc = half * 4 + q4
                    S.op("pe", lambda e, kc=kc, q4=q4, pb=pb: e.transpose(out=bk(pb)[:, q4 * 128:(q4 + 1) * 128],
                                                                        in_=xn32s[xs_][:, kc * 128:(kc + 1) * 128], identity=ident_f),
                         reads=[("xn32", xs_), "ident_f"], writes=[("bank", pb)])
                for q4 in range(4):
                    kc = half * 4 + q4
                    if half == 0:
                        S.op("act", lambda e, kc=kc, q4=q4, pb=pb: e.activation(
                            out=h2f[hi][:, kc, :], in_=bk(pb)[:, q4 * 128:(q4 + 1) * 128], func=AF.Identity, scale=G2[:, kc:kc + 1], bias=SH2[:, kc:kc + 1]),
                            reads=[("bank", pb), "G2", "MOD2"], writes=[("h2f", hi)])
                    else:
                        S.op("dve", lambda e, kc=kc, q4=q4, pb=pb: e.tensor_scalar(
                            out=h2f[hi][:, kc, :], in0=bk(pb)[:, q4 * 128:(q4 + 1) * 128], scalar1=G2[:, kc:kc + 1], scalar2=SH2[:, kc:kc + 1],
                            op0=ALU.mult, op1=ALU.add),
                            reads=[("bank", pb), "G2", "MOD2"], writes=[("h2f", hi)])
            for kc in range(8):
                S.op("pe", lambda e, kc=kc: e.matmul(bk(2)[:, 0:36], lhsT=h2f[hi][:, kc, :], rhs=wrt_s[:, kc, :], start=(kc == 0), stop=(kc == 7)),
                     reads=[("h2f", hi), "wrt_s"], writes=[("bank", 2)])
            S.op("dve", lambda e: e.tensor_tensor(out=LG[:, T, :], in0=bk(2)[:, 0:36], in1=brt_s, op=ALU.add),
                 reads=[("bank", 2), "brt_s"], writes=["LG"])

        r_front(0)
        for T in range(16):
            if T + 1 < 16:
                r_front(T + 1)
            r_back(T)
        BIG = 1.0e30
        gl = LG[:, :, 0:4]
        el = LG[:, :, 4:36]

        def rop(eng, fn, reads, writes):
            S.op(eng, fn, reads=reads, writes=writes)

        rop("dve", lambda e: e.reduce_max(out=r_gmax, in_=gl, axis=AX.X), ["LG"], ["r_gmax"])
        rop("dve", lambda e: e.tensor_tensor(out=r_gl, in0=gl, in1=r_gmax.unsqueeze(2).to_broadcast([128, 16, 4]), op=ALU.subtract),
            ["LG", "r_gmax"], ["r_gl"])
        rop("dve", lambda e: e.tensor_scalar(out=r_gmask, in0=r_gl, scalar1=0.0, scalar2=None, op0=ALU.is_ge), ["r_gl"], ["r_gmask"])
        rop("act", lambda e: e.activation(out=r_gl, in_=r_gl, func=AF.Exp, bias=zero_c, scale=1.0), ["r_gl", "zero_c"], ["r_gl"])
        rop("dve", lambda e: e.reduce_sum(out=r_gsum, in_=r_gl, axis=AX.X), ["r_gl"], ["r_gsum"])
        rop("dve", lambda e: e.reciprocal(out=r_gw, in_=r_gsum), ["r_gsum"], ["r_gw"])
        rop("dve", lambda e: e.tensor_scalar(out=r_gmask, in0=r_gmask, scalar1=-1.0, scalar2=BIG, op0=ALU.add, op1=ALU.mult), ["r_gmask"], ["r_gmask"])
        for g in range(4):
            rop("dve", lambda e, g=g: e.tensor_tensor(out=r_elm[:, :, g * 8:(g + 1) * 8], in0=el[:, :, g * 8:(g + 1) * 8],
                                                      in1=r_gmask[:, :, g:g + 1].to_broadcast([128, 16, 8]), op=ALU.add),
                ["LG", "r_gmask"], ["r_elm"])
        rop("dve", lambda e: e.reduce_max(out=r_m1, in_=r_elm, axis=AX.X), ["r_elm"], ["r_m1"])
        rop("dve", lambda e: e.tensor_tensor(out=r_mask1, in0=r_elm, in1=r_m1.unsqueeze(2).to_broadcast([128, 16, 32]), op=ALU.is_ge),
            ["r_elm", "r_m1"], ["r_mask1"])
        rop("dve", lambda e: e.scalar_tensor_tensor(out=r_elm2, in0=r_mask1, scalar=-BIG, in1=r_elm, op0=ALU.mult, op1=ALU.add),
            ["r_mask1", "r_elm"], ["r_elm2"])
        rop("dve", lambda e: e.reduce_max(out=r_m2, in_=r_elm2, axis=AX.X), ["r_elm2"], ["r_m2"])
        rop("dve", lambda e: e.tensor_tensor(out=r_mask2, in0=r_elm2, in1=r_m2.unsqueeze(2).to_broadcast([128, 16, 32]), op=ALU.is_ge),
            ["r_elm2", "r_m2"], ["r_mask2"])
        rop("dve", lambda e: e.tensor_tensor(out=r_d, in0=r_m2, in1=r_m1, op=ALU.subtract), ["r_m1", "r_m2"], ["r_d"])
        rop("act", lambda e: e.activation(out=r_d, in_=r_d, func=AF.Exp, bias=zero_c, scale=1.0), ["r_d", "zero_c"], ["r_d"])
        rop("dve", lambda e: e.tensor_scalar(out=r_w1, in0=r_d, scalar1=1.0, scalar2=None, op0=ALU.add), ["r_d"], ["r_w1"])
        rop("dve", lambda e: e.reciprocal(out=r_w1, in_=r_w1), ["r_w1"], ["r_w1"])
        rop("dve", lambda e: e.tensor_tensor(out=r_w1, in0=r_w1, in1=r_gw, op=ALU.mult), ["r_w1", "r_gw"], ["r_w1"])
        rop("dve", lambda e: e.tensor_tensor(out=r_w2, in0=r_w1, in1=r_d, op=ALU.mult), ["r_w1", "r_d"], ["r_w2"])
        for k, (mk, mkey, wk, wkey) in enumerate(((r_mask1, "r_mask1", r_w1, "r_w1"), (r_mask2, "r_mask2", r_w2, "r_w2"))):
            rop("dve", lambda e, mk=mk: e.tensor_tensor(out=r_tmp, in0=mk, in1=erow.unsqueeze(1).to_broadcast([128, 16, 32]), op=ALU.mult),
                [mkey, "erow"], ["r_tmp"])
            rop("dve", lambda e, k=k: e.reduce_sum(out=r_e[:, :, k], in_=r_tmp, axis=AX.X), ["r_tmp"], [("r_e", k)])
            rop("dve", lambda e, k=k, wk=wk: e.tensor_copy(out=wsel[:, :, k], in_=wk), [wkey], [("wsel", k)])
        rop("dve", lambda e: e.tensor_tensor(out=Mb, in0=r_mask1, in1=r_mask2, op=ALU.add), ["r_mask1", "r_mask2"], ["Mb"])
        for T in range(16):
            pb = 2 + (T % 2)
            for T2 in range(T + 1):
                S.op("pe", lambda e, T=T, T2=T2, pb=pb: e.matmul(bk(pb)[:, 0:32], lhsT=(utb if T2 == T else ones_b5), rhs=Mb[:, T2, :],
                                                                 start=(T2 == 0), stop=(T2 == T)),
                     reads=["Mb", "utb", "ones_b5"], writes=[("bank", pb)])
            S.op("act", lambda e, T=T, pb=pb: e.activation(out=posA[:, T, :], in_=bk(pb)[:, 0:32], func=AF.Copy),
                 reads=[("bank", pb)], writes=["posA"])
        for k, (mk, mkey) in enumerate(((r_mask1, "r_mask1"), (r_mask2, "r_mask2"))):
            rop("dve", lambda e, mk=mk: e.tensor_tensor(out=r_tmp, in0=mk, in1=posA, op=ALU.mult), [mkey, "posA"], ["r_tmp"])
            rop("dve", lambda e, k=k: e.reduce_sum(out=r_p[:, :, k], in_=r_tmp, axis=AX.X), ["r_tmp"], [("r_p", k)])
        rop("dve", lambda e: e.tensor_scalar(out=r_ov, in0=r_p, scalar1=float(CAP), scalar2=1.0e6, op0=ALU.is_ge, op1=ALU.mult),
            [("r_p", 0), ("r_p", 1)], ["r_ov"])
        rop("dve", lambda e: e.scalar_tensor_tensor(out=r_p, in0=r_e, scalar=float(CAP), in1=r_p, op0=ALU.mult, op1=ALU.add),
            [("r_e", 0), ("r_e", 1), ("r_p", 0), ("r_p", 1)], ["r_slot"])
        rop("dve", lambda e: e.tensor_scalar(out=r_p, in0=r_p, scalar1=float(NE * CAP - 1), scalar2=None, op0=ALU.min), ["r_slot", "r_ov"], ["r_slot"])
        rop("dve", lambda e: e.tensor_copy(out=idx_i, in_=r_p), ["r_slot"], ["idx_i"])
        rop("dve", lambda e: e.tensor_tensor(out=r_tmp, in0=r_mask1, in1=r_w1.unsqueeze(2).to_broadcast([128, 16, 32]), op=ALU.mult),
            ["r_mask1", "r_w1"], ["r_tmp"])
        rop("dve", lambda e: e.tensor_tensor(out=comb, in0=r_mask2, in1=r_w2.unsqueeze(2).to_broadcast([128, 16, 32]), op=ALU.mult),
            ["r_mask2", "r_w2"], ["comb"])
        rop("dve", lambda e: e.tensor_tensor(out=comb, in0=comb, in1=r_tmp, op=ALU.add), ["comb", "r_tmp"], ["comb"])
        for T in range(16):
            S.op("pe", lambda e, T=T: e.matmul(bk(4)[:, 0:32], lhsT=ones_b5, rhs=Mb[:, T, :], start=(T == 0), stop=(T == 15)),
                 reads=["Mb", "ones_b5"], writes=[("bank", 4)])
        rop("dve", lambda e: e.reduce_max(out=flag_f[:, 0:1], in_=bk(4)[:, 0:32], axis=AX.X), [("bank", 4)], ["flag_f"])
        rop("dve", lambda e: e.tensor_scalar(out=flag_f[:, 1:2], in0=flag_f[:, 0:1], scalar1=float(os.environ.get("MK_FLAGTHR", CAP)) + 0.5, scalar2=None, op0=ALU.is_ge),
            ["flag_f"], ["flag_f"])
        rop("dve", lambda e: e.tensor_copy(out=flag_i, in_=flag_f), ["flag_f"], ["flag_i"])
        if "comb" in dumps:
            dump_out["idx"] = (idx_i.rearrange("p a b -> p (a b)"), [128, 32], I32, ["idx_i"])
            dump_out["wsel"] = (wsel.rearrange("p a b -> p (a b)"), [128, 32], F32, [("wsel", 0), ("wsel", 1)])
            dump_out["lg"] = (LG.rearrange("p a b -> p (a b)"), [128, 16 * 36], F32, ["LG"])

        S.barrier()
        S_main = S
        S = Sched(nc)
        S_sparse = S
        subscheds.append(S)
        A.release(m5b)
        hs_t = [A.alloc([2, 1024], BF16) for _ in range(2)]
        h2e = [A.alloc([8, HCAP], BF16) for _ in range(2)]
        NW = 2
        wge = [A.alloc([8, 256], BF16) for _ in range(NW)]
        wue = [A.alloc([8, 256], BF16) for _ in range(NW)]
        wde = [A.alloc([2, 1024], BF16) for _ in range(NW)]
        sgs = [A.alloc([512], BF16) for _ in range(2)]
        def _xnb_f32(i):
            return XNB[:, 4 * i:4 * i + 4, :].rearrange("p a b -> p (a b)").bitcast(F32).rearrange("p (a b) -> p a b", a=8)
        stg_g = [_xnb_f32(0), _xnb_f32(1)]
        stg_u = [_xnb_f32(2), _xnb_f32(3)]
        xnb_keys = [("XNB", T_) for T_ in range(16)]
        stg_d = [A.alloc([2, 1024], F32) for _ in range(2)]
        actT = [A.alloc([2, HCAP], BF16) for _ in range(2)]
        ysb = [A.alloc([1024], F32) for _ in range(2)]
        ysc = [0]
        gbuf = [A.alloc([1024], F32) for _ in range(3)] + list(xt)
        NSLOT = NE * CAP
        for T in range(16):
            for k in range(2):
                S.dma("pool", lambda e, T=T, k=k: e.indirect_dma_start(
                    out=HS[:, :], out_offset=bass.IndirectOffsetOnAxis(ap=idx_i[:, T, k:k + 1], axis=0),
                    in_=XNB[:, T, :], in_offset=None),
                    reads=[("XNB", T), "idx_i"], writes=["HS"], semkey="HSsc")
        def stage_w(e_):
            sg_ = e_ % 2
            S.dma("sp", lambda e: e.dma_start(out=stg_g[sg_], in_=wg[e_].rearrange("(kc p) f -> p kc f", p=128)),
                  writes=[("stg_g", sg_)] + (xnb_keys if e_ < 2 else []), semkey=("stg_g", sg_))
            S.dma("sp", lambda e: e.dma_start(out=stg_u[sg_], in_=wu[e_].rearrange("(kc p) f -> p kc f", p=128)),
                  writes=[("stg_u", sg_)] + (xnb_keys if e_ < 2 else []), semkey=("stg_u", sg_))
            S.dma("sp", lambda e: e.dma_start(out=stg_d[sg_], in_=wd[e_].rearrange("(fc p) d -> p fc d", p=128)),
                  writes=[("stg_d", sg_)], semkey=("stg_d", sg_))

        def casts_w(e_):
            ws, sg_ = e_ % NW, e_ % 2
            S.op("act", lambda e: e.activation(out=wge[ws], in_=stg_g[sg_], func=AF.Copy),
                 reads=[("stg_g", sg_)], writes=[("wge", ws)])
            S.op("pool", lambda e: e.tensor_copy(out=wue[ws], in_=stg_u[sg_]),
                 reads=[("stg_u", sg_)], writes=[("wue", ws)])
            S.op("dve", lambda e: e.tensor_tensor(out=wde[ws], in0=stg_d[sg_], in1=GFrep.unsqueeze(1).to_broadcast([128, 2, 1024]), op=ALU.mult),
                 reads=[("stg_d", sg_), ("Grep", id(GFrep), 0), ("Grep", id(GFrep), 1)], writes=[("wde", ws)])

        def body_A(k):
            e_, hf, i2 = k // 2, k % 2, k % 2
            tb0 = 0 if i2 == 0 else 6
            for half in range(2):
                pv = bkb(tb0 + half).rearrange("p (a b c) -> p a b c", a=4, b=2)
                for q4 in range(4):
                    kc = half * 4 + q4
                    for st_ in range(2):
                        S.op("pe", lambda e, kc=kc, q4=q4, st_=st_, pv=pv: e.transpose(
                            out=pv[:, q4, st_, :], in_=hs_t[i2][:, st_, kc * 128:(kc + 1) * 128], identity=ident_b),
                            reads=[("hs_t", i2), "ident_b"], writes=[("bank", tb0 + half)])
                for q4 in range(4):
                    kc = half * 4 + q4
                    S.op("act", lambda e, kc=kc, q4=q4, pv=pv: e.activation(
                        out=h2e[i2][:, kc, :], in_=pv[:, q4].rearrange("p a b -> p (a b)"), func=AF.Identity,
                        scale=G2[:, kc:kc + 1], bias=SH2[:, kc:kc + 1]),
                        reads=[("bank", tb0 + half), "G2", "MOD2"], writes=[("h2e", i2)])

        def body_A_load(k):
            e_, hf, i2 = k // 2, k % 2, k % 2
            S.dma("sp", lambda e: e.dma_start(out=hs_t[i2], in_=HS[e_ * CAP + hf * HCAP:e_ * CAP + (hf + 1) * HCAP, :].rearrange("(st p) d -> p st d", p=128)),
                  reads=["HS"], writes=[("hs_t", i2)], semkey=("hs_t", i2))

        def body_B(k):
            e_, i2 = k // 2, k % 2
            ws = e_ % NW
            for fc in range(2):
                for kc in range(8):
                    S.op("pe", lambda e, kc=kc, fc=fc: e.matmul(
                        bk(2)[:, fc * HCAP:(fc + 1) * HCAP], lhsT=wge[ws][:, kc, fc * 128:(fc + 1) * 128], rhs=h2e[i2][:, kc, :],
                        start=(kc == 0), stop=(kc == 7)),
                        reads=[("wge", ws), ("h2e", i2)], writes=[("bank", 2)])
                for kc in range(8):
                    S.op("pe", lambda e, kc=kc, fc=fc: e.matmul(
                        bk(3)[:, fc * HCAP:(fc + 1) * HCAP], lhsT=wue[ws][:, kc, fc * 128:(fc + 1) * 128], rhs=h2e[i2][:, kc, :],
                        start=(kc == 0), stop=(kc == 7)),
                        reads=[("wue", ws), ("h2e", i2)], writes=[("bank", 3)])
            S.op("act", lambda e: e.activation(out=sgs[i2], in_=bk(2), func=AF.Silu), reads=[("bank", 2)], writes=[("sgs", i2)])
            S.op("dve", lambda e: e.tensor_tensor(out=actT[i2].rearrange("p a b -> p (a b)"), in0=bk(3), in1=sgs[i2], op=ALU.mult),
                 reads=[("bank", 3), ("sgs", i2)], writes=[("actT", i2)])

        def body_C(k):
            e_, hf, i2 = k // 2, k % 2, k % 2
            ws = e_ % NW
            for st_ in range(2):
                yi = ysc[0] % 2
                ysc[0] += 1
                for dh in range(2):
                    pb = 4 + dh
                    for fc in range(2):
                        S.op("pe", lambda e, st_=st_, dh=dh, fc=fc, pb=pb: e.matmul(
                            bk(pb), lhsT=actT[i2][:, fc, st_ * 128:(st_ + 1) * 128], rhs=wde[ws][:, fc, dh * 512:(dh + 1) * 512],
                            start=(fc == 0), stop=(fc == 1)),
                            reads=[("actT", i2), ("wde", ws)], writes=[("bank", pb)])
                    if dh == 0:
                        S.op("act", lambda e, pb=pb, yi=yi: e.activation(out=ysb[yi][:, 0:512], in_=bk(pb), func=AF.Copy),
                             reads=[("bank", pb)], writes=[("ysb", yi)])
                    else:
                        S.op("dve", lambda e, pb=pb, yi=yi: e.tensor_copy(out=ysb[yi][:, 512:1024], in_=bk(pb)),
                             reads=[("bank", pb)], writes=[("ysb", yi)])
                S.dma("act", lambda e, st_=st_, yi=yi: e.dma_start(
                    out=YS[e_ * CAP + hf * HCAP + st_ * 128:e_ * CAP + hf * HCAP + (st_ + 1) * 128, :], in_=ysb[yi]),
                    reads=[("ysb", yi)], writes=[("YS", e_, hf, st_)], semkey=("YSw", yi))

        NBODY = 2 * NE
        body_A_load(0)
        body_A_load(1)
        stage_w(0)
        stage_w(1)
        casts_w(0)
        body_A(0)
        for k in range(NBODY):
            e_ = k // 2
            if k + 2 < NBODY:
                body_A_load(k + 2)
            if k % 2 == 0 and e_ >= 1:
                if e_ + 1 < NE:
                    stage_w(e_ + 1)
                casts_w(e_)
            if k + 1 < NBODY:
                body_A(k + 1)
            body_B(k)
            if k >= 1:
                body_C(k - 1)
        body_C(NBODY - 1)
        gc_ = 0
        for T in range(16):
            for k in range(2):
                gi = gc_ % len(gbuf)
                gc_ += 1
                S.dma("pool", lambda e, T=T, k=k, gi=gi: e.indirect_dma_start(
                    out=gbuf[gi], out_offset=None, in_=YS[:, :], in_offset=bass.IndirectOffsetOnAxis(ap=idx_i[:, T, k:k + 1], axis=0),
                    ),
                    reads=[("YS", e2, h2_, s2_) for e2 in range(NE) for h2_ in range(2) for s2_ in range(2)] + ["idx_i"], writes=[("gbuf", gi)], semkey=("gbuf", gi))
                S.op("dve", lambda e, T=T, k=k, gi=gi: e.scalar_tensor_tensor(
                    out=X1v(T), in0=gbuf[gi], scalar=wsel[:, T, k:k + 1], in1=X1v(T), op0=ALU.mult, op1=ALU.add),
                    reads=[("gbuf", gi), ("wsel", k), ("X1", T)], writes=[("X1", T)])
        S.barrier()
        sparse_top = A.top
        S = Sched(nc)
        S_dense = S
        subscheds.append(S)
        A.release(m5b)
        d_xn32 = A.alloc([1024], F32)
        d_h2f = [A.alloc([8, 128], F32) for _ in range(2)]
        d_h2T = XNB.rearrange("p a b -> p (a b)").rearrange("p (a b) -> p a b", a=8)
        d_actT = A.alloc([2, 2, TOWN], BF16)
        d_NW = 2
        d_wge = [A.alloc([8, 256], BF16) for _ in range(d_NW)]
        d_wue = [A.alloc([8, 256], BF16) for _ in range(d_NW)]
        d_wde = [A.alloc([2, 1024], BF16) for _ in range(d_NW)]
        d_combb = A.alloc([16, 32], BF16)
        d_combT = A.alloc([TOWN], BF16)
        d_SEL = A.alloc([32, 128], BF16)
        d_crs = [A.alloc([512], BF16) for _ in range(2)]
        d_sgs = [A.alloc([512], BF16) for _ in range(2)]
        d_tts = [A.alloc([512], BF16) for _ in range(2)]
        S.op("dve", lambda e: e.tensor_copy(out=d_SEL[0:32], in_=ident_b[0:32, 0:32].unsqueeze(2).to_broadcast([32, 32, 128])),
             reads=["ident_b"], writes=["SEL"])
        S.op("dve", lambda e: e.tensor_copy(out=d_combb, in_=comb), reads=["comb"], writes=["combb"])
        for T in range(16):
            rc_, rk = rstd_col(X1v(T), 128, [("X1", T)], D)
            S.op("dve", lambda e, T=T, rc_=rc_: e.tensor_scalar(out=d_xn32, in0=X1v(T), scalar1=rc_, scalar2=None, op0=ALU.mult),
                 reads=[("X1", T), rk], writes=["xn32"])
            hi = T % 2
            for half in range(2):
                pb = 0 + half
                for q4 in range(4):
                    kc = half * 4 + q4
                    S.op("pe", lambda e, kc=kc, q4=q4, pb=pb: e.transpose(out=bk(pb)[:, q4 * 128:(q4 + 1) * 128], in_=d_xn32[:, kc * 128:(kc + 1) * 128],
                                                                        identity=ident_f),
                         reads=["xn32", "ident_f"], writes=[("bank", pb)])
                for q4 in range(4):
                    kc = half * 4 + q4
                    S.op("act", lambda e, kc=kc, q4=q4, pb=pb, hi=hi: e.activation(
                        out=d_h2f[hi][:, kc, :], in_=bk(pb)[:, q4 * 128:(q4 + 1) * 128], func=AF.Identity, scale=G2[:, kc:kc + 1], bias=SH2[:, kc:kc + 1]),
                        reads=[("bank", pb), "G2", "MOD2"], writes=[("h2f", hi)])
            S.op("pool", lambda e, T=T, hi=hi: e.tensor_copy(out=d_h2T[:, :, T * 128:(T + 1) * 128], in_=d_h2f[hi]),
                 reads=[("h2f", hi)], writes=["h2T"])
        for T in range(16):
            pv = bkb(3)
            S.op("pe", lambda e, T=T, pv=pv: e.transpose(out=pv[0:32, 0:128], in_=d_combb[:, T, :], identity=ident_b),
                 reads=["combb", "ident_b"], writes=[("bank", 3)])
            S.op("act", lambda e, T=T, pv=pv: e.activation(out=d_combT[0:32, T * 128:(T + 1) * 128], in_=pv[0:32, 0:128], func=AF.Copy),
                 reads=[("bank", 3)], writes=["combT"])
        d_pgc = 0
        d_crc_ = [0]
        for e_ in range(NE):
            ws = e_ % d_NW
            es = e_ % 2
            S.dma("pool", lambda e, e_=e_, ws=ws: e.dma_start(out=d_wge[ws], in_=wg[e_].rearrange("(kc p) f -> p kc f", p=128)),
                  writes=[("wge", ws)], semkey=("wge", ws))
            S.dma("pool", lambda e, e_=e_, ws=ws: e.dma_start(out=d_wue[ws], in_=wu[e_].rearrange("(kc p) f -> p kc f", p=128)),
                  writes=[("wue", ws)], semkey=("wue", ws))
            S.dma("pool", lambda e, e_=e_, ws=ws: e.dma_start(out=d_wde[ws], in_=wd[e_].rearrange("(fc p) d -> p fc d", p=128)),
                  writes=[("wde", ws)], semkey=("wde", ws))
            for fc in range(2):
                S.op("dve", lambda e, ws=ws, fc=fc: e.tensor_tensor(out=d_wde[ws][:, fc, :], in0=d_wde[ws][:, fc, :], in1=GFrep, op=ALU.mult),
                     reads=[("wde", ws), ("Grep", id(GFrep), 0), ("Grep", id(GFrep), 1)], writes=[("wde", ws)])
            for tb in range(4):
                ci = d_crc_[0] % 2
                d_crc_[0] += 1
                S.op("pe", lambda e, e_=e_, tb=tb: e.matmul(bk(4), lhsT=d_SEL[0:32, e_, :], rhs=d_combT[0:32, tb * 512:(tb + 1) * 512], start=True, stop=True),
                     reads=["SEL", "combT"], writes=[("bank", 4)])
                S.op("act", lambda e, ci=ci: e.activation(out=d_crs[ci], in_=bk(4), func=AF.Copy), reads=[("bank", 4)], writes=[("crs", ci)])
                for fc in range(2):
                    pi = d_pgc % 2
                    d_pgc += 1
                    pg_, pu_ = 0 + 2 * pi, 1 + 2 * pi
                    for kc in range(8):
                        S.op("pe", lambda e, kc=kc, ws=ws, fc=fc, tb=tb, pg_=pg_: e.matmul(
                            bk(pg_), lhsT=d_wge[ws][:, kc, fc * 128:(fc + 1) * 128], rhs=d_h2T[:, kc, tb * 512:(tb + 1) * 512], start=(kc == 0), stop=(kc == 7)),
                            reads=[("wge", ws), "h2T"], writes=[("bank", pg_)])
                    for kc in range(8):
                        S.op("pe", lambda e, kc=kc, ws=ws, fc=fc, tb=tb, pu_=pu_: e.matmul(
                            bk(pu_), lhsT=d_wue[ws][:, kc, fc * 128:(fc + 1) * 128], rhs=d_h2T[:, kc, tb * 512:(tb + 1) * 512], start=(kc == 0), stop=(kc == 7)),
                            reads=[("wue", ws), "h2T"], writes=[("bank", pu_)])
                    S.op("act", lambda e, pi=pi, pg_=pg_: e.activation(out=d_sgs[pi], in_=bk(pg_), func=AF.Silu), reads=[("bank", pg_)], writes=[("sgs", pi)])
                    S.op("dve", lambda e, pi=pi, pu_=pu_: e.tensor_tensor(out=d_tts[pi], in0=bk(pu_), in1=d_sgs[pi], op=ALU.mult),
                         reads=[("bank", pu_), ("sgs", pi)], writes=[("tts", pi)])
                    S.op("pool", lambda e, pi=pi, ci=ci, es=es, fc=fc, tb=tb: e.tensor_tensor(
                        out=d_actT[:, es, fc, tb * 512:(tb + 1) * 512], in0=d_tts[pi], in1=d_crs[ci], op=ALU.mult),
                        reads=[("tts", pi), ("crs", ci)], writes=[("actT", es)])
            if e_ % 2 == 1:
                for T in range(16):
                    for dh in range(2):
                        pb = 5 + dh
                        n = 0
                        for es2 in range(2):
                            ws2 = (e_ - 1 + es2) % d_NW
                            for fc in range(2):
                                S.op("pe", lambda e, es2=es2, ws2=ws2, fc=fc, T=T, dh=dh, pb=pb, n=n: e.matmul(
                                    bk(pb), lhsT=d_actT[:, es2, fc, T * 128:(T + 1) * 128], rhs=d_wde[ws2][:, fc, dh * 512:(dh + 1) * 512],
                                    start=(n == 0), stop=(n == 3)),
                                    reads=[("actT", es2), ("wde", ws2)], writes=[("bank", pb)])
                                n += 1
                        S.op("dve", lambda e, T=T, dh=dh, pb=pb: e.tensor_tensor(
                            out=X1v(T)[:, dh * 512:(dh + 1) * 512], in0=bk(pb), in1=X1v(T)[:, dh * 512:(dh + 1) * 512], op=ALU.add),
                            reads=[("bank", pb), ("X1", T)], writes=[("X1", T)])
        S.barrier()
        A.top = max(A.top, sparse_top)
        S = S_main
        for eng_ in ENGS:
            def _branch(h, eng_=eng_):
                with h.register("moe_flag_" + eng_) as rg:
                    h.reg_load(rg, flag_i[0:1, 1:2])
                    with h.If_eq(rg, 0):
                        S_sparse._run(eng_, h)
                    with h.Else():
                        S_dense._run(eng_, h)
                return None
            S.op(eng_, _branch, reads=["flag_i"], no_ins=True)
        S.barrier()
        A.release(m5)
        FG = A.alloc([1024], F32)
        ot = [A.alloc([1024], F32) for _ in range(2)]
        ld(FG, gfin.partition_broadcast(128), "FG")
        for T in range(16):
            rc_, rk = rstd_col(X1v(T), 128, [("X1", T)], D)
            oi = T % 2
            S.op("dve", lambda e, T=T, rc_=rc_, oi=oi: e.scalar_tensor_tensor(out=ot[oi], in0=X1v(T), scalar=rc_, in1=FG, op0=ALU.mult, op1=ALU.mult),
                 reads=[("X1", T), rk, "FG"], writes=[("ot", oi)])
            S.dma("sp", lambda e, T=T, oi=oi: e.dma_start(out=out[T * 128:(T + 1) * 128, :], in_=ot[oi]),
                  reads=[("ot", oi)], writes=[("out", T)], semkey=("out", oi))
        return do_dumps_and_finish()


def prep_inputs(inputs):
    x = np.asarray(inputs["x"], np.float32)
    c = np.asarray(inputs["c"], np.float32)
    positions = np.asarray(inputs["positions"], np.int32)
    w_in = np.asarray(inputs["w_in"], np.float32)[0]
    idx = np.arange(512).reshape(8, 2, 32)[:, ::-1, :].reshape(512)
    winx = np.ascontiguousarray(np.concatenate([w_in, w_in[:, idx], w_in[:, 512 + idx]], axis=1))
    b_ada = np.asarray(inputs["b_ada"], np.float32)
    bchunks = b_ada[0].reshape(48, 128)
    b_fm = np.ascontiguousarray(bchunks[list(range(0, 16)) + list(range(24, 40))].T)
    fm = lambda v: np.ascontiguousarray(np.asarray(v, np.float32).reshape(8, 128).T)
    lamv = np.concatenate([np.asarray(inputs[k], np.float32)[0] for k in ("lambda_q1", "lambda_k1", "lambda_q2", "lambda_k2")])[None, :]
    cw = np.asarray(inputs["conv_w"], np.float32)[0]
    cwT = np.ascontiguousarray(cw.reshape(3, 4, 128).transpose(2, 1, 0).reshape(128, 12))
    wrt = np.ascontiguousarray(np.concatenate([np.asarray(inputs["w_group_router"], np.float32)[0],
                                               np.asarray(inputs["w_expert_router"], np.float32)[0]], axis=1))
    brt = np.concatenate([np.asarray(inputs["b_group_router"], np.float32)[0], np.asarray(inputs["b_expert_router"], np.float32)[0]])[None, :]
    d_ = np.arange(128) % 64
    invf = (10000.0 ** (-(d_ % 32).astype(np.float64) / 32.0)).astype(np.float32)[:, None]
    sgn = np.where(d_ < 32, -1.0, 1.0).astype(np.float32)[:, None]
    kidx = (np.arange(32)[None, :] * 128 + np.arange(128)[:, None]).astype(np.float32)
    shared = dict(
        w_ada=np.ascontiguousarray(np.asarray(inputs["w_ada"], np.float32)[0]), b_fm=b_fm, b_ada=np.ascontiguousarray(b_ada),
        gmix=fm(inputs["norm_mix_g"]), gffn=fm(inputs["norm_ffn_g"]), gfin=np.asarray(inputs["final_norm_g"], np.float32)[None, :],
        winx=winx, lamv=np.ascontiguousarray(lamv), subg=np.asarray(inputs["subln_g"], np.float32).reshape(1, 128),
        subgc=np.ascontiguousarray(np.asarray(inputs["subln_g"], np.float32).reshape(128, 1)), cwT=cwT,
        wua=np.ascontiguousarray(np.asarray(inputs["w_up_att"], np.float32)[0]),
        wuc=np.ascontiguousarray(np.asarray(inputs["w_up_conv"], np.float32)[0]),
        wout=np.ascontiguousarray(np.asarray(inputs["w_out"], np.float32)[0]), wrt=wrt, brt=np.ascontiguousarray(brt),
        wg=np.ascontiguousarray(np.asarray(inputs["w_gate"], np.float32)[0]), wu=np.ascontiguousarray(np.asarray(inputs["w_up"], np.float32)[0]),
        wd=np.ascontiguousarray(np.asarray(inputs["w_down"], np.float32)[0]), invf=invf, sgn=sgn, kidx=kidx,
        erow=np.arange(32, dtype=np.float32)[None, :],
        ut=(np.arange(128)[:, None] < np.arange(128)[None, :]).astype(np.float32))
    in_maps = []
    for cid in range(8):
        b, half = cid // 2, cid % 2
        chunks = OWN_CHUNKS[half]
        tok = np.concatenate([np.arange(cc * 512, (cc + 1) * 512) for cc in chunks])
        xhalo = np.zeros((8, D), np.float32)
        hvv = np.zeros((1, 8), np.float32)
        for i, cc in enumerate(chunks):
            if cc > 0:
                xhalo[2 * i:2 * i + 2] = x[b, cc * 512 - 2:cc * 512]
                hvv[0, 2 * i:2 * i + 2] = 1.0
        m = dict(shared)
        m.update(xo=np.ascontiguousarray(x[b, tok]), xs=np.ascontiguousarray(x[b]), xh=xhalo, hv=hvv,
                 cT=np.ascontiguousarray(c[b].reshape(8, 128).T), pos_s=np.ascontiguousarray(positions[b][None, :]),
                 pos_o=np.ascontiguousarray(positions[b, tok][None, :]), qidx=tok.astype(np.float32)[None, :])
        in_maps.append(m)
    return in_maps


_PROG = {}


def kernel(**inputs):
    in_maps = prep_inputs(inputs)
    if "full" not in _PROG:
        _PROG["full"] = build_program()
    nc = _PROG["full"]
    res = run_bass_kernel_spmd(nc, in_maps, core_ids=list(range(8)))
    outp = np.zeros((NB, SEQ, D), np.float32)
    for cid in range(8):
        b, half = cid // 2, cid % 2
        o = np.asarray(res.results[cid]["out"], np.float32)
        for i, cc in enumerate(OWN_CHUNKS[half]):
            outp[b, cc * 512:(cc + 1) * 512] = o[i * 512:(i + 1) * 512]
    return outp
```

```python
import math
import os
from contextlib import ExitStack

import numpy as np
import concourse.bass as bass
import concourse.mybir as mybir
from concourse.bass_utils import run_bass_kernel_spmd


ENGS = ("pe", "act", "dve", "pool", "sp")


class _Op:
    __slots__ = ("eng", "fn", "deps", "is_dma", "semkey", "count", "sig", "signal",
                 "clock", "raw_deps", "idx", "no_ins")

    def __init__(self, eng, fn):
        self.eng = eng
        self.fn = fn
        self.deps = []
        self.raw_deps = set()
        self.is_dma = False
        self.semkey = None
        self.count = 0
        self.sig = 0
        self.signal = False
        self.clock = None
        self.no_ins = False


class Sched:
    _ninst = 0

    def __init__(self, nc):
        self.nc = nc
        self.tag = "k%d" % Sched._ninst
        Sched._ninst += 1
        self.ops = []
        self.last_writer = {}
        self.readers = {}
        self.dma_count = {}

    cut = None
    force = False

    def _add(self, o, reads, writes):
        if self.cut is not None and len(self.ops) >= self.cut and not self.force:
            return o
        deps = {}
        for k in reads:
            w = self.last_writer.get(k)
            if w is not None:
                deps[id(w)] = w
                o.raw_deps.add(id(w))
        for k in writes:
            w = self.last_writer.get(k)
            if w is not None:
                if not (o.is_dma and w.is_dma and o.semkey == w.semkey):
                    deps[id(w)] = w
            for r in self.readers.get(k, ()):
                if r is not o:
                    deps[id(r)] = r
        for k in reads:
            self.readers.setdefault(k, []).append(o)
        for k in writes:
            self.last_writer[k] = o
            self.readers[k] = []
        o.deps = list(deps.values())
        o.idx = len(self.ops)
        self.ops.append(o)
        return o

    def op(self, eng, fn, reads=(), writes=(), no_ins=False):
        o = _Op(eng, fn)
        o.no_ins = no_ins
        return self._add(o, reads, writes)

    def dma(self, queue, fn, reads=(), writes=(), semkey=None):
        o = _Op(queue, fn)
        o.is_dma = True
        o.semkey = semkey
        self.dma_count[semkey] = self.dma_count.get(semkey, 0) + 1
        o.count = self.dma_count[semkey]
        return self._add(o, reads, writes)

    def finalize(self, stack):
        nc = self.nc
        for o in self.ops:
            kept = []
            for d in o.deps:
                if d.is_dma:
                    kept.append(d)
                    continue
                if d.eng == o.eng and not o.is_dma:
                    if o.eng == "pe":
                        continue
                    if id(d) not in o.raw_deps:
                        continue
                kept.append(d)
                d.signal = True
            o.deps = kept
        cnt = {e: 0 for e in ENGS}
        for o in self.ops:
            if not o.is_dma and o.signal:
                cnt[o.eng] += 1
                o.sig = cnt[o.eng]
        self.sig_total = cnt
        esem = {e: stack.enter_context(nc.semaphore("s_%s_%s" % (self.tag, e))) for e in ENGS}
        dsem = {}
        for k in self.dma_count:
            dsem[k] = stack.enter_context(nc.semaphore("d_%s_%s" % (self.tag, str(k))))
        self.esem, self.dsem = esem, dsem
        known = {e: {} for e in ENGS}
        prog = {e: [] for e in ENGS}
        nwait = 0
        for o in self.ops:
            kn = known[o.eng]
            need = {}
            for d in o.deps:
                if d.is_dma:
                    name, val = ("d", d.semkey), 16 * d.count
                else:
                    name, val = ("e", d.eng), d.sig
                if kn.get(name, 0) >= val:
                    continue
                if need.get(name, 0) < val:
                    need[name] = val
            for d in o.deps:
                if d.clock:
                    for n2, v2 in d.clock.items():
                        if kn.get(n2, 0) < v2:
                            kn[n2] = v2
            for name, val in need.items():
                if kn.get(name, 0) < val:
                    kn[name] = val
            waits = []
            for name, val in need.items():
                sem = dsem[name[1]] if name[0] == "d" else esem[name[1]]
                waits.append((sem, val))
                nwait += 1
            if o.is_dma:
                o.clock = dict(kn)
                o.clock[("d", o.semkey)] = max(o.clock.get(("d", o.semkey), 0), 0)
            elif o.signal:
                o.clock = dict(kn)
                o.clock[("e", o.eng)] = o.sig
            prog[o.eng].append((waits, o))
        self.prog = prog
        self.nwait = nwait
        return prog

    def emit(self, block):
        nc = self.nc
        esem, dsem = self.esem, self.dsem

        def run(e, handle):
            for waits, o in self.prog[e]:
                for sem, val in waits:
                    handle.wait_ge(sem, val)
                ins = o.fn(handle)
                if ins is None:
                    continue
                if o.is_dma:
                    ins.then_inc(dsem[o.semkey], 16)
                elif o.signal:
                    ins.then_inc(esem[e], 1)

        self._run = run
        if block is None:
            return

        @block.tensor
        def _(h):
            run("pe", h)

        @block.scalar
        def _(h):
            run("act", h)

        @block.vector
        def _(h):
            run("dve", h)

        @block.gpsimd
        def _(h):
            run("pool", h)

        @block.sync
        def _(h):
            run("sp", h)

    def barrier(self):
        last = {}
        for o in self.ops:
            if getattr(o, "no_ins", False):
                continue
            if o.is_dma:
                last[("d", o.semkey)] = o
            else:
                last[("e", o.eng)] = o
        deps = list(last.values())
        for e in ENGS:
            b = _Op(e, lambda h: None)
            b.no_ins = True
            b.deps = list(deps)
            b.raw_deps = set(id(d) for d in deps)
            b.idx = len(self.ops)
            self.ops.append(b)


F32 = mybir.dt.float32
BF16 = mybir.dt.bfloat16
I32 = mybir.dt.int32
AF = mybir.ActivationFunctionType
ALU = mybir.AluOpType
AX = mybir.AxisListType

D = 1024
SEQ = 4096
NB = 4
TOWN = 2048
NE = 32
FF = 256
EPS = 1e-6
LAMBDA_INIT = 0.8 - 0.6 * math.exp(0.0)
TWO_PI = 2.0 * math.pi
C1 = 6.28125
C2 = TWO_PI - C1
OWN_CHUNKS = ([0, 3, 4, 7], [1, 2, 5, 6])
ARENA_BYTES = 206 * 1024
CAP = 512
HCAP = 256


class Arena:
    def __init__(self, ap):
        self.ap = ap
        self.top = 0
        self.limit = ap.shape[1] * 2

    def mark(self):
        return self.top

    def release(self, m):
        self.top = m

    def alloc(self, shape, dt):
        sz = 4 if dt in (F32, I32) else 2
        n = 1
        for s in shape:
            n *= s
        nbytes = (n * sz + 63) // 64 * 64
        off = self.top
        self.top += nbytes
        assert self.top <= self.limit, ("SBUF arena overflow", self.top, self.limit)
        v = self.ap[:, off // 2: off // 2 + (n * sz) // 2]
        if sz == 4:
            v = v.bitcast(dt)
        if len(shape) == 2:
            return v.rearrange("p (a b) -> p a b", a=shape[0])
        if len(shape) == 3:
            return v.rearrange("p (a b c) -> p a b c", a=shape[0], b=shape[1])
        return v


def build_program(stop_after=99, dumps=()):
    nc = bass.Bass("TRN2", target_bir_lowering=False)
    dt_in = {}

    def din(name, shape, dt=F32):
        dt_in[name] = nc.dram_tensor(name, list(shape), dt, kind="ExternalInput").ap()
        return dt_in[name]

    xo = din("xo", [TOWN, D])
    xs = din("xs", [SEQ, D])
    xh = din("xh", [8, D])
    hv = din("hv", [1, 8])
    cT = din("cT", [128, 8])
    pos_s = din("pos_s", [1, SEQ], I32)
    pos_o = din("pos_o", [1, TOWN], I32)
    qidx = din("qidx", [1, TOWN])
    kidx = din("kidx", [128, 32])
    invf = din("invf", [128, 1])
    sgn = din("sgn", [128, 1])
    w_ada = din("w_ada", [D, 6 * D])
    b_fm = din("b_fm", [128, 32])
    b_ada = din("b_ada", [1, 6 * D])
    gmix = din("gmix", [128, 8])
    gffn = din("gffn", [128, 8])
    gfin = din("gfin", [1, D])
    winx = din("winx", [D, 6144])
    lamv = din("lamv", [1, 256])
    subg = din("subg", [1, 128])
    subgc = din("subgc", [128, 1])
    cwT = din("cwT", [128, 12])
    wua = din("wua", [512, D])
    wuc = din("wuc", [512, D])
    wout = din("wout", [D, D])
    wrt = din("wrt", [D, 36])
    brt = din("brt", [1, 36])
    wg = din("wg", [NE, D, FF])
    wu = din("wu", [NE, D, FF])
    wd = din("wd", [NE, FF, D])
    erow_d = din("erow", [1, 32])
    ut_d = din("ut", [128, 128])
    out = nc.dram_tensor("out", [TOWN, D], F32, kind="ExternalOutput").ap()
    sgd = nc.dram_tensor("sgd", [4, 4, 128, 2048], BF16).ap()
    HS = nc.dram_tensor("hs_scratch", [NE * CAP, D], BF16).ap()
    YS = nc.dram_tensor("ys_scratch", [NE * CAP, D], F32).ap()
    dump_out = {}

    st = ExitStack()
    with st:
        arena_t = st.enter_context(nc.sbuf_tensor("arena", [128, ARENA_BYTES // 2], BF16))
        A = Arena(arena_t[:, :])
        banks = [st.enter_context(nc.psum_tensor("bank%d" % i, [128, 512], F32)) for i in range(8)]

        def bk(i):
            return banks[i][:, :]

        def bkb(i):
            return banks[i][:, :].bitcast(BF16)

        subscheds = []
        S = Sched(nc)
        if os.environ.get("MK_CUT"):
            S.cut = int(os.environ["MK_CUT"])
        uid = [0]

        def K(name):
            uid[0] += 1
            return (name, uid[0])

        KT = A.alloc([4, 4, 1024], BF16)
        VA = A.alloc([32, 4, 130], BF16)
        ident_f = A.alloc([128], F32)
        ident_b = A.alloc([128], BF16)
        cact = A.alloc([8], F32)
        cTs = A.alloc([8], F32)
        MOD = A.alloc([32], F32)
        bfm = A.alloc([32], F32)
        G1 = A.alloc([8], F32)
        G2 = A.alloc([8], F32)
        gmix_s = A.alloc([8], F32)
        gffn_s = A.alloc([8], F32)
        invf_s = A.alloc([1], F32)
        sgn_s = A.alloc([1], F32)
        kidx_s = A.alloc([32], F32)
        cw_s = A.alloc([12], F32)
        eps_c = A.alloc([1], F32)
        zero_c = A.alloc([1], F32)
        lam_s = A.alloc([4], F32)
        SUBG = A.alloc([128], F32)
        subgc_s = A.alloc([1], F32)
        GMrep = A.alloc([1024], BF16)
        GFrep = A.alloc([1024], BF16)
        stat = A.alloc([160], F32)
        rstd = A.alloc([160], F32)
        lnv = A.alloc([160], F32)
        hv_s = A.alloc([8], F32)
        sqj = A.alloc([1024], BF16)
        XT_N = 2
        xt = [A.alloc([1024], F32) for _ in range(XT_N)]
        xn = [A.alloc([1024], BF16) for _ in range(2)]
        persist_mark = A.mark()

        def X1v(T):
            j, t = T // 4, T % 4
            if t < 2:
                base = KT[:, j].rearrange("p h k -> p (h k)")[:, t * 2048:(t + 1) * 2048]
            else:
                base = VA[:, 8 * j:8 * j + 8].rearrange("p a h k -> p (a h k)")[:, (t - 2) * 2048:(t - 1) * 2048]
            return base.bitcast(F32)

        def X1key(T):
            j, t = T // 4, T % 4
            return ("KT", j) if t < 2 else ("VA", j)

        def ld(dst, src, key, q="sp"):
            S.dma(q, lambda e, d=dst, s=src: e.dma_start(out=d, in_=s), writes=[key], semkey=key)

        S.op("pool", lambda e: e.memset(ident_f, 0.0), writes=["ident_f"])
        S.op("pool", lambda e: e.affine_select(out=ident_f, in_=ident_f, pattern=[[-1, 128]],
                                               compare_op=ALU.not_equal, fill=1.0, base=0, channel_multiplier=1),
             reads=["ident_f"], writes=["ident_f"])
        S.op("dve", lambda e: e.tensor_copy(out=ident_b, in_=ident_f), reads=["ident_f"], writes=["ident_b"])
        S.op("dve", lambda e: e.memset(eps_c, EPS), writes=["eps_c"])
        S.op("dve", lambda e: e.memset(zero_c, 0.0), writes=["zero_c"])
        S.op("dve", lambda e: e.memset(stat, 0.0), writes=[("stat", c_) for c_ in range(160)])
        S.op("pool", lambda e: e.memset(VA[:, :, :, 128:130], 1.0), writes=[("VA", j) for j in range(4)])
        ld(cTs, cT, "cTs")
        ld(bfm, b_fm, "bfm")
        ld(gmix_s, gmix, "gmix_s")
        ld(gffn_s, gffn, "gffn_s")
        ld(invf_s, invf, "invf_s")
        ld(sgn_s, sgn, "sgn_s")
        ld(kidx_s, kidx, "kidx_s")
        ld(cw_s, cwT, "cw_s")
        ld(hv_s, hv.partition_broadcast(128), "hv_s")
        ld(SUBG, subg.partition_broadcast(128), "SUBG")
        ld(subgc_s, subgc, "subgc_s")

        mA = A.mark()
        wa = [A.alloc([8, 512], F32) for _ in range(2)]
        crep = A.alloc([8, 128], F32)
        brow = A.alloc([512], F32)
        lamr = A.alloc([256], F32)
        lamp = A.alloc([128], F32)
        S.op("act", lambda e: e.activation(out=cact, in_=cTs, func=AF.Silu), reads=["cTs"], writes=["cact"])
        for kc in range(8):
            S.op("dve", lambda e, kc=kc: e.tensor_copy(out=crep[:, kc, :], in_=cact[:, kc:kc + 1].to_broadcast([128, 128])),
                 reads=["cact"], writes=["crep"])
        ld(lamr, lamv.partition_broadcast(128), "lamr")
        S.op("dve", lambda e: e.tensor_tensor(out=lamp.rearrange("p (a b) -> p a b", a=2),
                                              in0=lamr.rearrange("p (a b c) -> p a b c", a=2, b=2)[:, :, 0, :],
                                              in1=lamr.rearrange("p (a b c) -> p a b c", a=2, b=2)[:, :, 1, :], op=ALU.mult),
             reads=["lamr"], writes=["lamp"])
        S.op("dve", lambda e: e.reduce_sum(out=lam_s[:, 2:4], in_=lamp.rearrange("p (a b) -> p a b", a=2), axis=AX.X),
             reads=["lamp"], writes=["lam23"])
        S.op("act", lambda e: e.activation(out=lam_s[:, 2:4], in_=lam_s[:, 2:4], func=AF.Exp, bias=zero_c, scale=1.0),
             reads=["lam23", "zero_c"], writes=["lam23"])
        S.op("dve", lambda e: e.tensor_tensor(out=lam_s[:, 0:1], in0=lam_s[:, 2:3], in1=lam_s[:, 3:4], op=ALU.subtract),
             reads=["lam23"], writes=["lam0"])
        S.op("dve", lambda e: e.tensor_scalar(out=lam_s[:, 1:2], in0=lam_s[:, 0:1], scalar1=LAMBDA_INIT, scalar2=-1.0, op0=ALU.add, op1=ALU.mult),
             reads=["lam0"], writes=["nlam"])
        S.op("pool", lambda e: e.tensor_scalar(out=SUBG, in0=SUBG, scalar1=1.0 - LAMBDA_INIT, scalar2=None, op0=ALU.mult),
             reads=["SUBG"], writes=["SUBG"])
        S.op("pool", lambda e: e.tensor_scalar(out=subgc_s, in0=subgc_s, scalar1=1.0 - LAMBDA_INIT, scalar2=None, op0=ALU.mult),
             reads=["subgc_s"], writes=["subgc_s"])

        wada_v = w_ada.rearrange("(kc p) c -> p kc c", p=128)
        ct_order = [0, 1, 2, 3, 6, 7, 8, 9, 4, 5, 10, 11]
        fm_col = {0: 0, 1: 4, 2: 8, 3: 12, 6: 16, 7: 20, 8: 24, 9: 28}
        pm = bk(0)
        first_fm = [True]
        def adaln_tile(i, pbank):
            ct = ct_order[i]
            slot = i % 2
            S.dma("sp", lambda e: e.dma_start(out=wa[slot], in_=wada_v[:, :, ct * 512:(ct + 1) * 512]),
                  writes=[("wa", slot)], semkey=("wa", slot))
            if ct in fm_col:
                c0 = fm_col[ct]
                for jj in range(4):
                    for kc in range(8):
                        S.op("pe", lambda e, jj=jj, kc=kc: e.matmul(
                            bk(pbank)[:, jj:jj + 1], lhsT=wa[slot][:, kc, jj * 128:(jj + 1) * 128], rhs=cact[:, kc:kc + 1],
                            start=(kc == 0), stop=(kc == 7)),
                            reads=[("wa", slot), "cact"], writes=[("bank", pbank)])
                mkey = "MOD1" if c0 < 16 else "MOD2"
                S.op("dve", lambda e: e.tensor_tensor(out=MOD[:, c0:c0 + 4], in0=bk(pbank)[:, 0:4], in1=bfm[:, c0:c0 + 4], op=ALU.add),
                     reads=[("bank", pbank), "bfm"], writes=[mkey, ("MODp", ct)])
            else:
                half = 0 if ct in (4, 10) else 1
                dst = GMrep if ct in (4, 5) else GFrep
                for kc in range(8):
                    S.op("pe", lambda e, kc=kc: e.matmul(
                        bk(pbank), lhsT=crep[:, kc, :], rhs=wa[slot][:, kc, :], start=(kc == 0), stop=(kc == 7)),
                        reads=[("wa", slot), "crep"], writes=[("bank", pbank)])
                S.dma("sp", lambda e: e.dma_start(out=brow, in_=b_ada[:, ct * 512:(ct + 1) * 512].partition_broadcast(128)),
                      writes=["brow"], semkey="brow")
                S.op("dve", lambda e: e.tensor_tensor(
                    out=dst[:, half * 512:(half + 1) * 512], in0=bk(pbank), in1=brow, op=ALU.add),
                    reads=[("bank", pbank), "brow"], writes=[("Grep", id(dst), half)])

        for i in range(4):
            adaln_tile(i, i % 2)
        S.op("dve", lambda e: e.scalar_tensor_tensor(out=G1, in0=MOD[:, 8:16], scalar=1.0, in1=gmix_s, op0=ALU.add, op1=ALU.mult),
             reads=["MOD1", "gmix_s"], writes=["G1"])
        SH1 = MOD[:, 0:8]
        SH2 = MOD[:, 16:24]

        if "mod" in dumps:
            dump_out["mod"] = (MOD, [128, 32], F32, ["MOD1", "MOD2"])
            dump_out["g1"] = (G1, [128, 8], F32, ["G1"])
            dump_out["lam"] = (lam_s, [128, 4], F32, ["lam0", "nlam"])
            dump_out["gmrep"] = (GMrep, [128, 1024], BF16, [("Grep", id(GMrep), 0), ("Grep", id(GMrep), 1)])

        statc = [0]
        xtc = [0]
        xnc = [0]
        ptrc = [0]

        def rstd_col(src_ap, npart, src_keys, n_feat):
            c = statc[0]
            statc[0] += 1
            assert c < 160
            kk = ("stat", c)
            S.op("act", lambda e: e.activation(out=sqj[0:npart, 0:src_ap.shape[1]], in_=src_ap, func=AF.Square,
                                               accum_out=stat[0:npart, c:c + 1]),
                 reads=src_keys, writes=[kk, "sqj"])
            S.op("act", lambda e: e.activation(out=lnv[0:npart, c:c + 1], in_=stat[0:npart, c:c + 1], func=AF.Ln,
                                               bias=eps_c[0:npart, :], scale=1.0 / n_feat),
                 reads=[kk, "eps_c"], writes=[("ln", c)])
            S.op("act", lambda e: e.activation(out=rstd[0:npart, c:c + 1], in_=lnv[0:npart, c:c + 1], func=AF.Exp,
                                               bias=zero_c[0:npart, :], scale=-0.5),
                 reads=[("ln", c), "zero_c"], writes=[("rstd", c)])
            return rstd[0:npart, c:c + 1], ("rstd", c)

        evc = [0]

        def norm_front(src_dram, npart):
            slot = xtc[0] % XT_N
            xtc[0] += 1
            xk = ("xt", slot)
            S.dma("sp", lambda e: e.dma_start(out=xt[slot][0:npart, :], in_=src_dram), writes=[xk], semkey=xk)
            rc, rk = rstd_col(xt[slot][0:npart, :], npart, [xk], D)
            ns = xnc[0] % 2
            xnc[0] += 1
            nk = ("xn", ns)
            S.op("dve", lambda e: e.tensor_scalar(out=xn[ns][0:npart, :], in0=xt[slot][0:npart, :], scalar1=rc, scalar2=None, op0=ALU.mult),
                 reads=[xk, rk], writes=[nk])
            return (ns, nk, npart)

        def norm_back(fr, dst_kc, dst_rng, dst_key, Gs, SHs, gkeys, ptr_banks, evt):
            ns, nk, npart = fr
            pb = ptr_banks[ptrc[0] % len(ptr_banks)]
            ptrc[0] += 1
            pk_ = ("bank", pb)
            pv = bkb(pb).rearrange("p (a b) -> p a b", a=8)
            for kc in range(8):
                S.op("pe", lambda e, kc=kc: e.transpose(out=pv[:, kc, 0:npart], in_=xn[ns][0:npart, kc * 128:(kc + 1) * 128],
                                                        identity=ident_b[0:npart, 0:npart]),
                     reads=[nk, "ident_b"], writes=[pk_])
            for kc in range(8):
                S.op("act", lambda e, kc=kc: e.activation(out=dst_kc(kc), in_=pv[:, kc, 0:npart], func=AF.Identity,
                                                          scale=Gs[:, kc:kc + 1], bias=SHs[:, kc:kc + 1]),
                     reads=[pk_] + gkeys, writes=[dst_key])

        def trig_tables(pos_dram, n0, tabs, tkey, tmp):
            posi, ang, u, nf = tmp
            ni = posi
            r = u
            S.dma("sp", lambda e: e.dma_start(out=posi, in_=pos_dram[:, n0:n0 + 512].partition_broadcast(128)),
                  writes=["posi"], semkey="posi")
            S.op("dve", lambda e: e.tensor_scalar(out=ang, in0=posi, scalar1=invf_s, scalar2=None, op0=ALU.mult),
                 reads=["posi", "invf_s"], writes=["ang"])
            for which in range(2):
                add = 0.0 if which == 0 else math.pi / 2
                S.op("dve", lambda e, add=add: e.tensor_scalar(out=u, in0=ang, scalar1=add, scalar2=1.0 / TWO_PI, op0=ALU.add, op1=ALU.mult),
                     reads=["ang"], writes=["u"])
                S.op("dve", lambda e: e.tensor_copy(out=ni, in_=u), reads=["u"], writes=["posi"])
                S.op("dve", lambda e: e.tensor_copy(out=nf, in_=ni), reads=["posi"], writes=["nf"])
                S.op("dve", lambda e, add=add: e.tensor_scalar(out=r, in0=ang, scalar1=add, scalar2=None, op0=ALU.add),
                     reads=["ang"], writes=["u"])
                S.op("dve", lambda e: e.scalar_tensor_tensor(out=r, in0=nf, scalar=-C1, in1=r, op0=ALU.mult, op1=ALU.add),
                     reads=["nf", "u"], writes=["u"])
                S.op("dve", lambda e: e.scalar_tensor_tensor(out=r, in0=nf, scalar=-C2, in1=r, op0=ALU.mult, op1=ALU.add),
                     reads=["nf", "u"], writes=["u"])
                S.op("dve", lambda e: e.tensor_scalar(out=r, in0=r, scalar1=math.pi, scalar2=-math.pi, op0=ALU.min, op1=ALU.max),
                     reads=["u"], writes=["u"])
                if which == 0:
                    S.op("act", lambda e: e.activation(out=tabs[:, 1, :], in_=r, func=AF.Sin, scale=sgn_s, bias=zero_c),
                         reads=["u", "sgn_s", "zero_c"], writes=[tkey])
                else:
                    S.op("act", lambda e: e.activation(out=tabs[:, 0, :], in_=r, func=AF.Sin, scale=1.0, bias=zero_c),
                         reads=["u", "zero_c"], writes=[tkey])

        def wload(dst, c0, c1, key):
            S.dma("pool", lambda e: e.dma_start(out=dst, in_=winx.rearrange("(kc p) c -> p kc c", p=128)[:, :, c0:c1]),
                  writes=[key], semkey=key)

        def rope_proj(Wn, Wr, wkeys, hT_ap, hkeys, h, tabs, tkey, dst, dkey, pbanks, tmps, tmpkeys):
            pa, pr = pbanks
            for kc in range(8):
                S.op("pe", lambda e, kc=kc: e.matmul(bk(pa), lhsT=Wn[:, kc, h * 128:(h + 1) * 128], rhs=hT_ap[:, kc, :],
                                                     start=(kc == 0), stop=(kc == 7)),
                     reads=wkeys + hkeys, writes=[("bank", pa)])
            for kc in range(8):
                S.op("pe", lambda e, kc=kc: e.matmul(bk(pr), lhsT=Wr[:, kc, h * 128:(h + 1) * 128], rhs=hT_ap[:, kc, :],
                                                     start=(kc == 0), stop=(kc == 7)),
                     reads=wkeys + hkeys, writes=[("bank", pr)])
            ta, tb = tmps
            ka, kb = tmpkeys
            S.op("dve", lambda e: e.tensor_tensor(out=ta, in0=bk(pa), in1=tabs[:, 0, :], op=ALU.mult),
                 reads=[("bank", pa), tkey], writes=[ka])
            S.op("dve", lambda e: e.tensor_tensor(out=tb, in0=bk(pr), in1=tabs[:, 1, :], op=ALU.mult),
                 reads=[("bank", pr), tkey], writes=[kb])
            S.op("pool", lambda e: e.tensor_tensor(out=dst, in0=ta, in1=tb, op=ALU.add), reads=[ka, kb], writes=[dkey])

        def do_dumps_and_finish():
            S.force = True
            names = []
            for name, (ap, shape, dt, keys) in dump_out.items():
                if name not in dumps:
                    continue
                dd = nc.dram_tensor("dbg_" + name, list(shape), dt, kind="ExternalOutput").ap()
                S.dma("sp", lambda e, dd=dd, ap=ap: e.dma_start(out=dd, in_=ap), reads=list(keys), writes=["dbg_out"], semkey="dbg_out")
                names.append(name)
            S.op("sp", lambda e: None, reads=["dbg_out"] + [("out", T_) for T_ in range(16)])
            S.finalize(st)
            for sub in subscheds:
                sub.finalize(st)
                sub.emit(None)
            if os.environ.get("MK_VERBOSE"):
                print("ops", len(S.ops), "sig", S.sig_total, "waits", S.nwait, "sbuf top", A.top)
            with nc.Block() as block:
                S.emit(block)
            return nc

        if stop_after <= 0:
            S.barrier()
            return do_dumps_and_finish()

        m2 = A.mark()
        hTa = [A.alloc([8, 512], BF16) for _ in range(2)]
        Wk = A.alloc([8, 512], BF16)
        Wkr = A.alloc([8, 512], BF16)
        Wv = A.alloc([8, 512], BF16)
        tabs2 = [A.alloc([2, 512], F32) for _ in range(2)]
        trig_tmp = (A.alloc([512], I32), A.alloc([512], F32), A.alloc([512], F32), A.alloc([512], F32))
        tmpa = [A.alloc([512], F32) for _ in range(2)]
        tmpb = [A.alloc([512], F32) for _ in range(2)]
        wload(Wk, 512, 1024, "Wk")
        wload(Wkr, 5632, 6144, "Wkr")
        wload(Wv, 1024, 1536, "Wv")
        evt = [A.alloc([4, 128], F32) for _ in range(2)]
        rc = [0]
        fr_state = [norm_front(xs[0:128, :], 128)]

        def p2_tile(tb, t):
            T = tb * 4 + t
            hs = tb % 2
            fr = fr_state[0]
            if T + 1 < 32:
                fr_state[0] = norm_front(xs[(T + 1) * 128:(T + 2) * 128, :], 128)
            norm_back(fr, lambda kc, hs=hs, t=t: hTa[hs][:, kc, t * 128:(t + 1) * 128],
                      lambda k0, k1, hs=hs, t=t: hTa[hs][:, k0:k1, t * 128:(t + 1) * 128],
                      ("hTa", hs), G1, SH1, ["G1", "MOD1"], [0, 1], evt)

        trig_tables(pos_s, 0, tabs2[0], ("tabs2", 0), trig_tmp)
        for t in range(4):
            p2_tile(0, t)
        for tb in range(8):
            hs = tb % 2
            hk = ("hTa", hs)
            tk = ("tabs2", hs)
            adaln_tile(4 + tb, 6)
            if tb == 7:
                S.op("dve", lambda e: e.scalar_tensor_tensor(out=G2, in0=MOD[:, 24:32], scalar=1.0, in1=gffn_s, op0=ALU.add, op1=ALU.mult),
                     reads=["MOD2", "gffn_s"], writes=["G2"])
            if tb + 1 < 8:
                trig_tables(pos_s, (tb + 1) * 512, tabs2[1 - hs], ("tabs2", 1 - hs), trig_tmp)
            jb, off = tb // 2, (tb % 2) * 512
            for h in range(4):
                i = rc[0] % 2
                rc[0] += 1
                rope_proj(Wk, Wkr, ["Wk", "Wkr"], hTa[hs], [hk], h, tabs2[hs], tk,
                          KT[:, jb, h, off:off + 512], ("KT", jb), (2 + 2 * i, 3 + 2 * i),
                          (tmpa[i], tmpb[i]), (("tmpa", i), ("tmpb", i)))
                if tb + 1 < 8:
                    p2_tile(tb + 1, h)
            for t in range(4):
                kt = tb * 4 + t
                pb = 6 + (t % 2)
                for kc in range(8):
                    S.op("pe", lambda e, kc=kc, t=t, pb=pb, hs=hs: e.matmul(
                        bk(pb), lhsT=hTa[hs][:, kc, t * 128:(t + 1) * 128], rhs=Wv[:, kc, :], start=(kc == 0), stop=(kc == 7)),
                        reads=[hk, "Wv"], writes=[("bank", pb)])
                S.op("act", lambda e, kt=kt, pb=pb: e.activation(
                    out=VA[:, kt, :, 0:128], in_=bk(pb).rearrange("p (h v) -> p h v", h=4), func=AF.Copy),
                    reads=[("bank", pb)], writes=[("VA", kt // 8)])
        if "kt" in dumps:
            dump_out["kt"] = (KT.rearrange("p a h k -> p (a h k)"), [128, 16384], BF16, [("KT", j) for j in range(4)])
            dump_out["va"] = (VA.rearrange("p a h k -> p (a h k)"), [128, 32 * 4 * 130], BF16, [("VA", j) for j in range(4)])
        if stop_after <= 2:
            S.barrier()
            return do_dumps_and_finish()

        S.barrier()
        A.release(mA)
        Qs = A.alloc([4, TOWN], BF16)
        convT = A.alloc([4, TOWN], BF16)
        m3 = A.mark()
        hTo = A.alloc([4, 8, 512], BF16)
        hTh = A.alloc([8, 8], BF16)
        wr = [A.alloc([8, 512], BF16) for _ in range(3)]
        m3s = A.mark()
        Uh = A.alloc([4, 8], F32)
        Ub = [A.alloc([514], F32) for _ in range(2)]
        yb = [A.alloc([512], F32) for _ in range(2)]
        ccS = [A.alloc([512], F32) for _ in range(2)]
        evt = [A.alloc([4, 128], F32) for _ in range(2)]
        fr_next = norm_front(xo[0:128, :], 128)
        for T in range(16):
            j, t = T // 4, T % 4
            fr = fr_next
            fr_next = norm_front(xo[(T + 1) * 128:(T + 2) * 128, :], 128) if T + 1 < 16 else norm_front(xh[:, :], 8)
            norm_back(fr, lambda kc, j=j, t=t: hTo[:, j, kc, t * 128:(t + 1) * 128],
                      lambda k0, k1, j=j, t=t: hTo[:, j, k0:k1, t * 128:(t + 1) * 128],
                      ("hTo", j), G1, SH1, ["G1", "MOD1"], [0, 1], evt)
        norm_back(fr_next, lambda kc: hTh[:, kc, :], lambda k0, k1: hTh[:, k0:k1, :], "hTh", G1, SH1, ["G1", "MOD1"], [0, 1], evt)
        wload(wr[0], 1536, 2048, ("wr", 0))
        wload(wr[1], 2048, 2560, ("wr", 1))
        wload(wr[2], 2560, 3072, ("wr", 2))
        for cch in range(4):
            for kc in range(8):
                S.op("pe", lambda e, kc=kc, cch=cch: e.matmul(bk(2)[:, 0:8], lhsT=wr[1][:, kc, cch * 128:(cch + 1) * 128], rhs=hTh[:, kc, :],
                                                              start=(kc == 0), stop=(kc == 7)),
                     reads=[("wr", 1), "hTh"], writes=[("bank", 2)])
            for kc in range(8):
                S.op("pe", lambda e, kc=kc, cch=cch: e.matmul(bk(3)[:, 0:8], lhsT=wr[2][:, kc, cch * 128:(cch + 1) * 128], rhs=hTh[:, kc, :],
                                                              start=(kc == 0), stop=(kc == 7)),
                     reads=[("wr", 2), "hTh"], writes=[("bank", 3)])
            S.op("act", lambda e, cch=cch: e.activation(out=Uh[:, cch, :], in_=bk(2)[:, 0:8], func=AF.Copy),
                 reads=[("bank", 2)], writes=[("Uh", cch)])
            S.op("dve", lambda e, cch=cch: e.tensor_tensor(out=Uh[:, cch, :], in0=bk(3)[:, 0:8], in1=Uh[:, cch, :], op=ALU.mult),
                 reads=[("bank", 3), ("Uh", cch)], writes=[("Uh", cch)])
            S.op("dve", lambda e, cch=cch: e.tensor_tensor(out=Uh[:, cch, :], in0=Uh[:, cch, :], in1=hv_s, op=ALU.mult),
                 reads=[("Uh", cch), "hv_s"], writes=[("Uh", cch)])
        cnt = 0
        for j in range(4):
            for cch in range(4):
                i = cnt % 2
                cnt += 1
                b_cc, b_cu, b_cb = 2 + 3 * i, 3 + 3 * i, 4 + 3 * i
                for (wi, pb) in ((1, b_cc), (2, b_cu), (0, b_cb)):
                    for kc in range(8):
                        S.op("pe", lambda e, kc=kc, wi=wi, pb=pb, cch=cch, j=j: e.matmul(
                            bk(pb), lhsT=wr[wi][:, kc, cch * 128:(cch + 1) * 128], rhs=hTo[:, j, kc, :], start=(kc == 0), stop=(kc == 7)),
                            reads=[("wr", wi), ("hTo", j)], writes=[("bank", pb)])
                S.op("act", lambda e, i=i, b_cc=b_cc: e.activation(out=ccS[i], in_=bk(b_cc), func=AF.Copy),
                     reads=[("bank", b_cc)], writes=[("ccS", i)])
                S.op("dve", lambda e, i=i, b_cu=b_cu: e.tensor_tensor(out=Ub[i][:, 2:514], in0=bk(b_cu), in1=ccS[i], op=ALU.mult),
                     reads=[("bank", b_cu), ("ccS", i)], writes=[("Ub", i)])
                S.op("pool", lambda e, i=i, cch=cch, j=j: e.tensor_copy(out=Ub[i][:, 0:2], in_=Uh[:, cch, 2 * j:2 * j + 2]),
                     reads=[("Uh", cch)], writes=[("Ubh", i)])
                S.op("act", lambda e, i=i, cch=cch: e.activation(out=yb[i], in_=Ub[i][:, 2:514], func=AF.Copy, scale=cw_s[:, cch * 3:cch * 3 + 1]),
                     reads=[("Ub", i), "cw_s"], writes=[("yb", i)])
                S.op("dve", lambda e, i=i, cch=cch: e.scalar_tensor_tensor(out=yb[i], in0=Ub[i][:, 1:513], scalar=cw_s[:, cch * 3 + 1:cch * 3 + 2],
                                                                          in1=yb[i], op0=ALU.mult, op1=ALU.add),
                     reads=[("Ub", i), ("Ubh", i), ("yb", i), "cw_s"], writes=[("yb", i)])
                S.op("dve", lambda e, i=i, cch=cch: e.scalar_tensor_tensor(out=yb[i], in0=Ub[i][:, 0:512], scalar=cw_s[:, cch * 3 + 2:cch * 3 + 3],
                                                                          in1=yb[i], op0=ALU.mult, op1=ALU.add),
                     reads=[("Ub", i), ("Ubh", i), ("yb", i), "cw_s"], writes=[("yb", i)])
                S.op("dve", lambda e, i=i, cch=cch, j=j, b_cb=b_cb: e.tensor_tensor(
                    out=convT[:, cch, j * 512:(j + 1) * 512], in0=bk(b_cb), in1=yb[i], op=ALU.mult),
                    reads=[("bank", b_cb), ("yb", i)], writes=["convT"])
        S.barrier()
        A.release(m3s)
        tabs3 = [A.alloc([2, 512], F32) for _ in range(2)]
        trig_tmp = (A.alloc([512], I32), A.alloc([512], F32), A.alloc([512], F32), A.alloc([512], F32))
        tmpa = [A.alloc([512], F32) for _ in range(2)]
        tmpb = [A.alloc([512], F32) for _ in range(2)]
        wload(wr[0], 0, 512, ("wr", 0))
        wload(wr[1], 5120, 5632, ("wr", 1))
        rc = [0]
        for j in range(4):
            ts = j % 2
            tk = ("tabs3", ts)
            trig_tables(pos_o, j * 512, tabs3[ts], tk, trig_tmp)
            for h in range(4):
                i = rc[0] % 2
                rc[0] += 1
                rope_proj(wr[0], wr[1], [("wr", 0), ("wr", 1)], hTo[:, j], [("hTo", j)], h, tabs3[ts], tk,
                          Qs[:, h, j * 512:(j + 1) * 512], "Qs", (2 + 2 * i, 3 + 2 * i),
                          (tmpa[i], tmpb[i]), (("tmpa", i), ("tmpb", i)))
        S.barrier()
        A.release(m3s)
        sgb = [A.alloc([4, 512], BF16) for _ in range(2)]
        gcnt = 0
        for g in range(4):
            ws = (2, 0, 1, 2)[g]
            wload(wr[ws], 3072 + g * 512, 3072 + (g + 1) * 512, ("wr", ws))
            for j in range(4):
                si = gcnt % 2
                gcnt += 1
                for dq in range(4):
                    pb = 2 + (dq % 4)
                    for kc in range(8):
                        S.op("pe", lambda e, kc=kc, ws=ws, dq=dq, pb=pb, j=j: e.matmul(
                            bk(pb), lhsT=wr[ws][:, kc, dq * 128:(dq + 1) * 128], rhs=hTo[:, j, kc, :], start=(kc == 0), stop=(kc == 7)),
                            reads=[("wr", ws), ("hTo", j)], writes=[("bank", pb)])
                    S.op("act", lambda e, si=si, dq=dq, pb=pb: e.activation(out=sgb[si][:, dq, :], in_=bk(pb), func=AF.Sigmoid,
                                                                             bias=zero_c, scale=1.0),
                         reads=[("bank", pb), "zero_c"], writes=[("sgb", si)])
                S.dma("sp", lambda e, si=si, g=g, j=j: e.dma_start(out=sgd[g, j].rearrange("p (a b) -> p a b", a=4), in_=sgb[si]),
                      reads=[("sgb", si)], writes=[("sgd", g, j)], semkey=("sgd", si))
        if "q" in dumps:
            dump_out["q"] = (Qs.rearrange("p h t -> p (h t)"), [128, 4 * TOWN], BF16, ["Qs"])
            dump_out["conv"] = (convT.rearrange("p h t -> p (h t)"), [128, 4 * TOWN], BF16, ["convT"])
        if stop_after <= 3:
            S.barrier()
            return do_dumps_and_finish()

        S.barrier()
        A.release(m3)
        m4 = A.mark()
        Wua = A.alloc([4, 1024], BF16)
        Wuc = A.alloc([4, 1024], BF16)
        Wo = A.alloc([8, 1024], BF16)
        pT = [A.alloc([512], BF16) for _ in range(4)]
        Qp = [A.alloc([2, 2, 256], BF16) for _ in range(2)]
        qrep2 = A.alloc([2, 2, 256], F32)
        ones_b = A.alloc([128], BF16)
        rL = A.alloc([512], F32)
        tO = A.alloc([512], F32)
        oo = A.alloc([256], F32)
        o2b = A.alloc([256], BF16)
        ors = A.alloc([256], F32)
        qpc = 0
        qslot = {}
        S.op("pool", lambda e: e.memset(ones_b, 1.0), writes=["ones_b"])
        for qs_ in range(2):
            S.op("pool", lambda e, qs_=qs_: e.memset(Qp[qs_], 0.0), writes=[("Qpz", qs_), ("Qp", qs_)])
        attTs = [A.alloc([4, 512], BF16) for _ in range(2)]
        carry = []
        xslot = {}
        jcount = 0
        mergedT = A.alloc([8, 512], BF16)
        m1b = [A.alloc([512], F32) for _ in range(2)]
        m2b = [A.alloc([512], F32) for _ in range(2)]
        sgl = [A.alloc([2, 512], BF16) for _ in range(3)]
        qrep = A.alloc([512], F32)
        S.dma("pool", lambda e: e.dma_start(out=Wua, in_=wua.rearrange("(kc p) c -> p kc c", p=128)), writes=["Wua"], semkey="Wua")
        S.dma("pool", lambda e: e.dma_start(out=Wuc, in_=wuc.rearrange("(kc p) c -> p kc c", p=128)), writes=["Wuc"], semkey="Wuc")
        S.dma("pool", lambda e: e.dma_start(out=Wo, in_=wout.rearrange("(kc p) c -> p kc c", p=128)), writes=["Wo"], semkey="Wo")
        wo_scaled = [False]
        stc = 0
        ptc = 0
        sgc = 0
        mc = 0
        for j in (3, 2, 1, 0):
            nkt = 8 * (j + 1)
            S.dma("sp", lambda e, j=j: e.dma_start(out=qrep, in_=qidx[:, j * 512:(j + 1) * 512].partition_broadcast(128)),
                  writes=["qrep"], semkey="qrep")
            S.op("dve", lambda e: e.tensor_copy(out=qrep2, in_=qrep.rearrange("p (a b) -> p a b", a=2).unsqueeze(2).to_broadcast([128, 2, 2, 256])),
                 reads=["qrep"], writes=["qrep2"])
            nkq = (nkt - 2, nkt)
            items = [(h, qh, kt) for h in range(4) for qh in range(2) for kt in range(nkq[qh])]
            LOOK = 3
            slots = {}
            asl = jcount % 2
            jcount += 1
            pending = []
            for step in range(len(items) + LOOK):
                while pending and pending[0][0] <= step:
                    pending.pop(0)[1]()
                if carry and step >= 2 and step % 2 == 0:
                    carry.pop(0)()
                if step < len(items):
                    h, qh, kt = items[step]
                    if qh == 0 and kt == 0:
                        qs_ = qpc % 2
                        qpc += 1
                        qslot[h] = qs_
                        for m in range(2):
                            S.op("pool", lambda e, m=m, h=h, j=j, qs_=qs_: e.tensor_copy(
                                out=Qp[qs_][m * 64:(m + 1) * 64, :, m, :],
                                in_=Qs[m * 64:(m + 1) * 64, h, j * 512:(j + 1) * 512].rearrange("p (a b) -> p a b", a=2)),
                                reads=["Qs", ("Qpz", qs_)], writes=[("Qp", qs_)])
                    qs_ = qslot[h]
                    sb_ = stc % 3
                    stc += 1
                    pi = ptc % 4
                    ptc += 1
                    slots[step] = pi
                    jb, ko = kt // 8, (kt % 8) * 128
                    S.op("pe", lambda e, sb_=sb_, jb=jb, ko=ko, h=h, qh=qh, qs_=qs_: e.matmul(
                        bk(sb_), lhsT=KT[:, jb, h, ko:ko + 128], rhs=Qp[qs_][:, qh].rearrange("p m q -> p (m q)"),
                        start=True, stop=True),
                        reads=[("KT", jb), ("Qp", qs_)], writes=[("bank", sb_)])
                    S.op("act", lambda e, sb_=sb_, pi=pi: e.activation(out=pT[pi], in_=bk(sb_), func=AF.Exp, bias=zero_c, scale=0.125),
                         reads=[("bank", sb_), "zero_c"], writes=[("pT", pi)])
                    if kt >= 8 * j:
                        S.op("dve", lambda e, pi=pi, kt=kt, qh=qh: e.scalar_tensor_tensor(
                            out=pT[pi], in0=qrep2[:, qh].rearrange("p m q -> p (m q)"), scalar=kidx_s[:, kt:kt + 1], in1=pT[pi],
                            op0=ALU.is_ge, op1=ALU.mult),
                            reads=[("pT", pi), "qrep2", "kidx_s"], writes=[("pT", pi)])
                if step - LOOK < 0:
                    continue
                h, qh, kt = items[step - LOOK]
                pi = slots.pop(step - LOOK)
                ai = (h * 2 + qh) % 2
                bo, bl = 3 + 2 * ai, 4 + 2 * ai
                nkt_ = nkq[qh]
                S.op("pe", lambda e, pi=pi, kt=kt, h=h, nkt=nkt_, bo=bo: e.matmul(
                    bk(bo), lhsT=VA[:, kt, h, 0:128], rhs=pT[pi], start=(kt == 0), stop=(kt == nkt - 1)),
                    reads=[("pT", pi), ("VA", kt // 8)], writes=[("bank", bo)])
                S.op("pe", lambda e, pi=pi, kt=kt, nkt=nkt_, bl=bl: e.matmul(
                    bk(bl), lhsT=ones_b, rhs=pT[pi], start=(kt == 0), stop=(kt == nkt - 1)),
                    reads=[("pT", pi), "ones_b"], writes=[("bank", bl)])
                if kt != nkt_ - 1:
                    continue
                S.op("dve", lambda e, bl=bl: e.reciprocal(out=rL, in_=bk(bl)), reads=[("bank", bl)], writes=["rL"])
                S.op("dve", lambda e, bo=bo: e.tensor_tensor(out=tO, in0=bk(bo), in1=rL, op=ALU.mult), reads=[("bank", bo), "rL"], writes=["tO"])
                S.op("dve", lambda e: e.scalar_tensor_tensor(out=oo, in0=tO[:, 256:512], scalar=lam_s[:, 1:2], in1=tO[:, 0:256],
                                                             op0=ALU.mult, op1=ALU.add),
                     reads=["tO", "nlam"], writes=["oo"])
                S.op("pool", lambda e: e.tensor_tensor(out=o2b, in0=oo, in1=oo, op=ALU.mult), reads=["oo"], writes=["o2b"])

                def _subln_tail(h=h, qh=qh, asl_=asl):
                    S.op("pe", lambda e: e.matmul(bk(7)[:, 0:256], lhsT=ones_b, rhs=o2b, start=True, stop=True),
                         reads=["o2b", "ones_b"], writes=[("bank", 7)])
                    S.op("act", lambda e: e.activation(out=ors, in_=bk(7)[:, 0:256], func=AF.Ln, bias=eps_c, scale=1.0 / 128),
                         reads=[("bank", 7), "eps_c"], writes=["ors"])
                    S.op("act", lambda e: e.activation(out=ors, in_=ors, func=AF.Exp, bias=zero_c, scale=-0.5), reads=["ors", "zero_c"], writes=["ors"])
                    S.op("dve", lambda e: e.scalar_tensor_tensor(out=attTs[asl_][:, h, qh * 256:(qh + 1) * 256], in0=oo, scalar=subgc_s, in1=ors,
                                                                 op0=ALU.mult, op1=ALU.mult),
                         reads=["oo", "ors", "subgc_s"], writes=[("attT", asl_)])
                pending.append((step + 5, _subln_tail))
            for _, fn_ in pending:
                fn_()
            pending = []
            while carry:
                carry.pop(0)()
            if not wo_scaled[0]:
                wo_scaled[0] = True
                for kc in range(8):
                    S.op("dve", lambda e, kc=kc: e.tensor_tensor(out=Wo[:, kc, :], in0=Wo[:, kc, :], in1=GMrep, op=ALU.mult),
                         reads=["Wo", ("Grep", id(GMrep), 0), ("Grep", id(GMrep), 1)], writes=["Wo"])
            overl = (j != 0)
            chunks = []
            for dc in range(8):
                si = sgc % 3
                sgc += 1
                mi = mc % 2
                mc += 1
                g_a, g_c, dq = dc // 4, 2 + dc // 4, dc % 4
                ba = 7 if overl else 6
                bc_ = 7

                def _ca(dc=dc, si=si, mi=mi, g_a=g_a, g_c=g_c, dq=dq, j=j, ba=ba, asl=asl):
                    S.dma("sp", lambda e: e.dma_start(out=sgl[si][:, 0, :], in_=sgd[g_a, j, :, dq * 512:(dq + 1) * 512]),
                          reads=[("sgd", g_a, j)], writes=[("sgl", si)], semkey=("sgl", si))
                    S.dma("sp", lambda e: e.dma_start(out=sgl[si][:, 1, :], in_=sgd[g_c, j, :, dq * 512:(dq + 1) * 512]),
                          reads=[("sgd", g_c, j)], writes=[("sgl", si)], semkey=("sgl", si))
                    for kc in range(4):
                        S.op("pe", lambda e, kc=kc: e.matmul(bk(ba), lhsT=Wua[:, kc, dc * 128:(dc + 1) * 128], rhs=attTs[asl][:, kc, :],
                                                             start=(kc == 0), stop=(kc == 3)),
                             reads=["Wua", ("attT", asl)], writes=[("bank", ba)])
                    S.op("dve", lambda e: e.tensor_tensor(out=m1b[mi], in0=bk(ba), in1=sgl[si][:, 0, :], op=ALU.mult),
                         reads=[("bank", ba), ("sgl", si)], writes=[("m1b", mi)])

                def _cc(dc=dc, si=si, mi=mi, j=j, bc_=bc_):
                    for kc in range(4):
                        S.op("pe", lambda e, kc=kc: e.matmul(bk(bc_), lhsT=Wuc[:, kc, dc * 128:(dc + 1) * 128], rhs=convT[:, kc, j * 512:(j + 1) * 512],
                                                             start=(kc == 0), stop=(kc == 3)),
                             reads=["Wuc", "convT"], writes=[("bank", bc_)])
                    S.op("dve", lambda e: e.tensor_tensor(out=m2b[mi], in0=bk(bc_), in1=sgl[si][:, 1, :], op=ALU.mult),
                         reads=[("bank", bc_), ("sgl", si)], writes=[("m2b", mi)])
                    S.op("pool", lambda e: e.tensor_tensor(out=mergedT[:, dc, :], in0=m1b[mi], in1=m2b[mi], op=ALU.add),
                         reads=[("m1b", mi), ("m2b", mi)], writes=["mergedT"])
                chunks.append(_ca)
                chunks.append(_cc)
            for t in range(4):
                T = j * 4 + t
                for dh in range(2):
                    pb = 7 if overl else 6 + dh

                    def _co(t=t, T=T, dh=dh, pb=pb):
                        if dh == 0:
                            slot = xtc[0] % XT_N
                            xtc[0] += 1
                            xslot[T] = slot
                            S.dma("sp", lambda e: e.dma_start(out=xt[slot], in_=xo[T * 128:(T + 1) * 128, :]),
                                  writes=[("xt", slot)], semkey=("xt", slot))
                        slot = xslot[T]
                        xk = ("xt", slot)
                        for kc in range(8):
                            S.op("pe", lambda e, kc=kc: e.matmul(
                                bk(pb), lhsT=mergedT[:, kc, t * 128:(t + 1) * 128], rhs=Wo[:, kc, dh * 512:(dh + 1) * 512],
                                start=(kc == 0), stop=(kc == 7)),
                                reads=["mergedT", "Wo"], writes=[("bank", pb)])
                        S.op("dve", lambda e: e.tensor_tensor(
                            out=X1v(T)[:, dh * 512:(dh + 1) * 512], in0=bk(pb), in1=xt[slot][:, dh * 512:(dh + 1) * 512], op=ALU.add),
                            reads=[("bank", pb), xk], writes=[X1key(T), ("X1", T)])
                    chunks.append(_co)
            if overl:
                carry = chunks
            else:
                for c_ in chunks:
                    c_()
        if "x1" in dumps:
            for T in (0, 5, 15):
                dump_out["x1_%d" % T] = (X1v(T), [128, 1024], F32, [("X1", T)])
        if stop_after <= 4:
            S.barrier()
            return do_dumps_and_finish()

        S.barrier()
        A.release(persist_mark)
        m5 = A.mark()
        XNB = A.alloc([16, 1024], BF16)
        idx_i = A.alloc([16, 2], I32)
        wsel = A.alloc([16, 2], F32)
        ones_b5 = A.alloc([128], BF16)
        utb = A.alloc([128], BF16)
        erow = A.alloc([32], F32)
        comb = A.alloc([16, 32], F32)
        flag_f = A.alloc([2], F32)
        flag_i = A.alloc([2], I32)
        m5b = A.mark()
        xn32s = [A.alloc([1024], F32) for _ in range(2)]
        h2f = [A.alloc([8, 128], F32) for _ in range(2)]
        LG = A.alloc([16, 36], F32)
        wrt_s = A.alloc([8, 36], F32)
        brt_s = A.alloc([36], F32)
        utf = A.alloc([128], F32)
        r_gmax = A.alloc([16], F32)
        r_gl = A.alloc([16, 4], F32)
        r_gsum = A.alloc([16], F32)
        r_gw = A.alloc([16], F32)
        r_gmask = A.alloc([16, 4], F32)
        r_elm = A.alloc([16, 32], F32)
        r_m1 = A.alloc([16], F32)
        r_mask1 = A.alloc([16, 32], F32)
        r_elm2 = A.alloc([16, 32], F32)
        r_m2 = A.alloc([16], F32)
        r_mask2 = A.alloc([16, 32], F32)
        r_d = A.alloc([16], F32)
        r_w1 = A.alloc([16], F32)
        r_w2 = A.alloc([16], F32)
        r_tmp = A.alloc([16, 32], F32)
        r_e = A.alloc([16, 2], F32)
        r_p = A.alloc([16, 2], F32)
        r_ov = A.alloc([16, 2], F32)
        Mb = A.alloc([16, 32], BF16)
        posA = A.alloc([16, 32], F32)
        ld(wrt_s, wrt.rearrange("(kc p) c -> p kc c", p=128), "wrt_s")
        ld(brt_s, brt.partition_broadcast(128), "brt_s")
        ld(erow, erow_d.partition_broadcast(128), "erow")
        ld(utf, ut_d, "utf")
        S.op("dve", lambda e: e.tensor_copy(out=utb, in_=utf), reads=["utf"], writes=["utb"])
        S.op("pool", lambda e: e.memset(ones_b5, 1.0), writes=["ones_b5"])
        def r_front(T):
            rc_, rk = rstd_col(X1v(T), 128, [("X1", T)], D)
            xs_ = T % 2
            S.op("dve", lambda e: e.tensor_scalar(out=xn32s[xs_], in0=X1v(T), scalar1=rc_, scalar2=None, op0=ALU.mult),
                 reads=[("X1", T), rk], writes=[("xn32", xs_)])
            S.op("pool", lambda e: e.tensor_copy(out=XNB[:, T, :], in_=xn32s[xs_]), reads=[("xn32", xs_)], writes=[("XNB", T)])

        def r_back(T):
            xs_ = T % 2
            hi = T % 2
            b0 = 0 if T % 2 == 0 else 4
            for half in range(2):
                pb = b0 + half
                for q4 in range(4):
                    kc = half * 4 + q4
                    S.op("pe", lambda e, kc=kc, q4=q4, pb=pb: e.transpose(out=bk(pb)[:, q4 * 128:(q4 + 1) * 128],
                                                                        in_=xn32s[xs_][:, kc * 128:(kc + 1) * 128], identity=ident_f),
                         reads=[("xn32", xs_), "ident_f"], writes=[("bank", pb)])
                for q4 in range(4):
                    kc = half * 4 + q4
                    if half == 0:
                        S.op("act", lambda e, kc=kc, q4=q4, pb=pb: e.activation(
                            out=h2f[hi][:, kc, :], in_=bk(pb)[:, q4 * 128:(q4 + 1) * 128], func=AF.Identity, scale=G2[:, kc:kc + 1], bias=SH2[:, kc:kc + 1]),
                            reads=[("bank", pb), "G2", "MOD2"], writes=[("h2f", hi)])
                    else:
                        S.op("dve", lambda e, kc=kc, q4=q4, pb=pb: e.tensor_scalar(
                            out=h2f[hi][:, kc, :], in0=bk(pb)[:, q4 * 128:(q4 + 1) * 128], scalar1=G2[:, kc:kc + 1], scalar2=SH2[:, kc:kc + 1],
                            op0=ALU.mult, op1=ALU.add),
                            reads=[("bank", pb), "G2", "MOD2"], writes=[("h2f", hi)])
            for kc in range(8):
                S.op("pe", lambda e, kc=kc: e.matmul(bk(2)[:, 0:36], lhsT=h2f[hi][:, kc, :], rhs=wrt_s[:, kc, :], start=(kc == 0), stop=(kc == 7)),
                     reads=[("h2f", hi), "wrt_s"], writes=[("bank", 2)])
            S.op("dve", lambda e: e.tensor_tensor(out=LG[:, T, :], in0=bk(2)[:, 0:36], in1=brt_s, op=ALU.add),
                 reads=[("bank", 2), "brt_s"], writes=["LG"])

        r_front(0)
        for T in range(16):
            if T + 1 < 16:
                r_front(T + 1)
            r_back(T)
        BIG = 1.0e30
        gl = LG[:, :, 0:4]
        el = LG[:, :, 4:36]

        def rop(eng, fn, reads, writes):
            S.op(eng, fn, reads=reads, writes=writes)

        rop("dve", lambda e: e.reduce_max(out=r_gmax, in_=gl, axis=AX.X), ["LG"], ["r_gmax"])
        rop("dve", lambda e: e.tensor_tensor(out=r_gl, in0=gl, in1=r_gmax.unsqueeze(2).to_broadcast([128, 16, 4]), op=ALU.subtract),
            ["LG", "r_gmax"], ["r_gl"])
        rop("dve", lambda e: e.tensor_scalar(out=r_gmask, in0=r_gl, scalar1=0.0, scalar2=None, op0=ALU.is_ge), ["r_gl"], ["r_gmask"])
        rop("act", lambda e: e.activation(out=r_gl, in_=r_gl, func=AF.Exp, bias=zero_c, scale=1.0), ["r_gl", "zero_c"], ["r_gl"])
        rop("dve", lambda e: e.reduce_sum(out=r_gsum, in_=r_gl, axis=AX.X), ["r_gl"], ["r_gsum"])
        rop("dve", lambda e: e.reciprocal(out=r_gw, in_=r_gsum), ["r_gsum"], ["r_gw"])
        rop("dve", lambda e: e.tensor_scalar(out=r_gmask, in0=r_gmask, scalar1=-1.0, scalar2=BIG, op0=ALU.add, op1=ALU.mult), ["r_gmask"], ["r_gmask"])
        for g in range(4):
            rop("dve", lambda e, g=g: e.tensor_tensor(out=r_elm[:, :, g * 8:(g + 1) * 8], in0=el[:, :, g * 8:(g + 1) * 8],
                                                      in1=r_gmask[:, :, g:g + 1].to_broadcast([128, 16, 8]), op=ALU.add),
                ["LG", "r_gmask"], ["r_elm"])
        rop("dve", lambda e: e.reduce_max(out=r_m1, in_=r_elm, axis=AX.X), ["r_elm"], ["r_m1"])
        rop("dve", lambda e: e.tensor_tensor(out=r_mask1, in0=r_elm, in1=r_m1.unsqueeze(2).to_broadcast([128, 16, 32]), op=ALU.is_ge),
            ["r_elm", "r_m1"], ["r_mask1"])
        rop("dve", lambda e: e.scalar_tensor_tensor(out=r_elm2, in0=r_mask1, scalar=-BIG, in1=r_elm, op0=ALU.mult, op1=ALU.add),
            ["r_mask1", "r_elm"], ["r_elm2"])
        rop("dve", lambda e: e.reduce_max(out=r_m2, in_=r_elm2, axis=AX.X), ["r_elm2"], ["r_m2"])
        rop("dve", lambda e: e.tensor_tensor(out=r_mask2, in0=r_elm2, in1=r_m2.unsqueeze(2).to_broadcast([128, 16, 32]), op=ALU.is_ge),
            ["r_elm2", "r_m2"], ["r_mask2"])
        rop("dve", lambda e: e.tensor_tensor(out=r_d, in0=r_m2, in1=r_m1, op=ALU.subtract), ["r_m1", "r_m2"], ["r_d"])
        rop("act", lambda e: e.activation(out=r_d, in_=r_d, func=AF.Exp, bias=zero_c, scale=1.0), ["r_d", "zero_c"], ["r_d"])
        rop("dve", lambda e: e.tensor_scalar(out=r_w1, in0=r_d, scalar1=1.0, scalar2=None, op0=ALU.add), ["r_d"], ["r_w1"])
        rop("dve", lambda e: e.reciprocal(out=r_w1, in_=r_w1), ["r_w1"], ["r_w1"])
        rop("dve", lambda e: e.tensor_tensor(out=r_w1, in0=r_w1, in1=r_gw, op=ALU.mult), ["r_w1", "r_gw"], ["r_w1"])
        rop("dve", lambda e: e.tensor_tensor(out=r_w2, in0=r_w1, in1=r_d, op=ALU.mult), ["r_w1", "r_d"], ["r_w2"])
        for k, (mk, mkey, wk, wkey) in enumerate(((r_mask1, "r_mask1", r_w1, "r_w1"), (r_mask2, "r_mask2", r_w2, "r_w2"))):
            rop("dve", lambda e, mk=mk: e.tensor_tensor(out=r_tmp, in0=mk, in1=erow.unsqueeze(1).to_broadcast([128, 16, 32]), op=ALU.mult),
                [mkey, "erow"], ["r_tmp"])
            rop("dve", lambda e, k=k: e.reduce_sum(out=r_e[:, :, k], in_=r_tmp, axis=AX.X), ["r_tmp"], [("r_e", k)])
            rop("dve", lambda e, k=k, wk=wk: e.tensor_copy(out=wsel[:, :, k], in_=wk), [wkey], [("wsel", k)])
        rop("dve", lambda e: e.tensor_tensor(out=Mb, in0=r_mask1, in1=r_mask2, op=ALU.add), ["r_mask1", "r_mask2"], ["Mb"])
        for T in range(16):
            pb = 2 + (T % 2)
            for T2 in range(T + 1):
                S.op("pe", lambda e, T=T, T2=T2, pb=pb: e.matmul(bk(pb)[:, 0:32], lhsT=(utb if T2 == T else ones_b5), rhs=Mb[:, T2, :],
                                                                 start=(T2 == 0), stop=(T2 == T)),
                     reads=["Mb", "utb", "ones_b5"], writes=[("bank", pb)])
            S.op("act", lambda e, T=T, pb=pb: e.activation(out=posA[:, T, :], in_=bk(pb)[:, 0:32], func=AF.Copy),
                 reads=[("bank", pb)], writes=["posA"])
        for k, (mk, mkey) in enumerate(((r_mask1, "r_mask1"), (r_mask2, "r_mask2"))):
            rop("dve", lambda e, mk=mk: e.tensor_tensor(out=r_tmp, in0=mk, in1=posA, op=ALU.mult), [mkey, "posA"], ["r_tmp"])
            rop("dve", lambda e, k=k: e.reduce_sum(out=r_p[:, :, k], in_=r_tmp, axis=AX.X), ["r_tmp"], [("r_p", k)])
        rop("dve", lambda e: e.tensor_scalar(out=r_ov, in0=r_p, scalar1=float(CAP), scalar2=1.0e6, op0=ALU.is_ge, op1=ALU.mult),
            [("r_p", 0), ("r_p", 1)], ["r_ov"])
        rop("dve", lambda e: e.scalar_tensor_tensor(out=r_p, in0=r_e, scalar=float(CAP), in1=r_p, op0=ALU.mult, op1=ALU.add),
            [("r_e", 0), ("r_e", 1), ("r_p", 0), ("r_p", 1)], ["r_slot"])
        rop("dve", lambda e: e.tensor_scalar(out=r_p, in0=r_p, scalar1=float(NE * CAP - 1), scalar2=None, op0=ALU.min), ["r_slot", "r_ov"], ["r_slot"])
        rop("dve", lambda e: e.tensor_copy(out=idx_i, in_=r_p), ["r_slot"], ["idx_i"])
        rop("dve", lambda e: e.tensor_tensor(out=r_tmp, in0=r_mask1, in1=r_w1.unsqueeze(2).to_broadcast([128, 16, 32]), op=ALU.mult),
            ["r_mask1", "r_w1"], ["r_tmp"])
        rop("dve", lambda e: e.tensor_tensor(out=comb, in0=r_mask2, in1=r_w2.unsqueeze(2).to_broadcast([128, 16, 32]), op=ALU.mult),
            ["r_mask2", "r_w2"], ["comb"])
        rop("dve", lambda e: e.tensor_tensor(out=comb, in0=comb, in1=r_tmp, op=ALU.add), ["comb", "r_tmp"], ["comb"])
        for T in range(16):
            S.op("pe", lambda e, T=T: e.matmul(bk(4)[:, 0:32], lhsT=ones_b5, rhs=Mb[:, T, :], start=(T == 0), stop=(T == 15)),
                 reads=["Mb", "ones_b5"], writes=[("bank", 4)])
        rop("dve", lambda e: e.reduce_max(out=flag_f[:, 0:1], in_=bk(4)[:, 0:32], axis=AX.X), [("bank", 4)], ["flag_f"])
        rop("dve", lambda e: e.tensor_scalar(out=flag_f[:, 1:2], in0=flag_f[:, 0:1], scalar1=float(os.environ.get("MK_FLAGTHR", CAP)) + 0.5, scalar2=None, op0=ALU.is_ge),
            ["flag_f"], ["flag_f"])
        rop("dve", lambda e: e.tensor_copy(out=flag_i, in_=flag_f), ["flag_f"], ["flag_i"])
        if "comb" in dumps:
            dump_out["idx"] = (idx_i.rearrange("p a b -> p (a b)"), [128, 32], I32, ["idx_i"])
            dump_out["wsel"] = (wsel.rearrange("p a b -> p (a b)"), [128, 32], F32, [("wsel", 0), ("wsel", 1)])
            dump_out["lg"] = (LG.rearrange("p a b -> p (a b)"), [128, 16 * 36], F32, ["LG"])

        S.barrier()
        S_main = S
        S = Sched(nc)
        S_sparse = S
        subscheds.append(S)
        A.release(m5b)
        hs_t = [A.alloc([2, 1024], BF16) for _ in range(2)]
        h2e = [A.alloc([8, HCAP], BF16) for _ in range(2)]
        NW = 2
        wge = [A.alloc([8, 256], BF16) for _ in range(NW)]
        wue = [A.alloc([8, 256], BF16) for _ in range(NW)]
        wde = [A.alloc([2, 1024], BF16) for _ in range(NW)]
        sgs = [A.alloc([512], BF16) for _ in range(2)]
        def _xnb_f32(i):
            return XNB[:, 4 * i:4 * i + 4, :].rearrange("p a b -> p (a b)").bitcast(F32).rearrange("p (a b) -> p a b", a=8)
        stg_g = [_xnb_f32(0), _xnb_f32(1)]
        stg_u = [_xnb_f32(2), _xnb_f32(3)]
        xnb_keys = [("XNB", T_) for T_ in range(16)]
        stg_d = [A.alloc([2, 1024], F32) for _ in range(2)]
        actT = [A.alloc([2, HCAP], BF16) for _ in range(2)]
        ysb = [A.alloc([1024], F32) for _ in range(2)]
        ysc = [0]
        gbuf = [A.alloc([1024], F32) for _ in range(3)] + list(xt)
        NSLOT = NE * CAP
        for T in range(16):
            for k in range(2):
                S.dma("pool", lambda e, T=T, k=k: e.indirect_dma_start(
                    out=HS[:, :], out_offset=bass.IndirectOffsetOnAxis(ap=idx_i[:, T, k:k + 1], axis=0),
                    in_=XNB[:, T, :], in_offset=None),
                    reads=[("XNB", T), "idx_i"], writes=["HS"], semkey="HSsc")
        def stage_w(e_):
            sg_ = e_ % 2
            S.dma("sp", lambda e: e.dma_start(out=stg_g[sg_], in_=wg[e_].rearrange("(kc p) f -> p kc f", p=128)),
                  writes=[("stg_g", sg_)] + (xnb_keys if e_ < 2 else []), semkey=("stg_g", sg_))
            S.dma("sp", lambda e: e.dma_start(out=stg_u[sg_], in_=wu[e_].rearrange("(kc p) f -> p kc f", p=128)),
                  writes=[("stg_u", sg_)] + (xnb_keys if e_ < 2 else []), semkey=("stg_u", sg_))
            S.dma("sp", lambda e: e.dma_start(out=stg_d[sg_], in_=wd[e_].rearrange("(fc p) d -> p fc d", p=128)),
                  writes=[("stg_d", sg_)], semkey=("stg_d", sg_))

        def casts_w(e_):
            ws, sg_ = e_ % NW, e_ % 2
            S.op("act", lambda e: e.activation(out=wge[ws], in_=stg_g[sg_], func=AF.Copy),
                 reads=[("stg_g", sg_)], writes=[("wge", ws)])
            S.op("pool", lambda e: e.tensor_copy(out=wue[ws], in_=stg_u[sg_]),
                 reads=[("stg_u", sg_)], writes=[("wue", ws)])
            S.op("dve", lambda e: e.tensor_tensor(out=wde[ws], in0=stg_d[sg_], in1=GFrep.unsqueeze(1).to_broadcast([128, 2, 1024]), op=ALU.mult),
                 reads=[("stg_d", sg_), ("Grep", id(GFrep), 0), ("Grep", id(GFrep), 1)], writes=[("wde", ws)])

        def body_A(k):
            e_, hf, i2 = k // 2, k % 2, k % 2
            tb0 = 0 if i2 == 0 else 6
            for half in range(2):
                pv = bkb(tb0 + half).rearrange("p (a b c) -> p a b c", a=4, b=2)
                for q4 in range(4):
                    kc = half * 4 + q4
                    for st_ in range(2):
                        S.op("pe", lambda e, kc=kc, q4=q4, st_=st_, pv=pv: e.transpose(
                            out=pv[:, q4, st_, :], in_=hs_t[i2][:, st_, kc * 128:(kc + 1) * 128], identity=ident_b),
                            reads=[("hs_t", i2), "ident_b"], writes=[("bank", tb0 + half)])
                for q4 in range(4):
                    kc = half * 4 + q4
                    S.op("act", lambda e, kc=kc, q4=q4, pv=pv: e.activation(
                        out=h2e[i2][:, kc, :], in_=pv[:, q4].rearrange("p a b -> p (a b)"), func=AF.Identity,
                        scale=G2[:, kc:kc + 1], bias=SH2[:, kc:kc + 1]),
                        reads=[("bank", tb0 + half), "G2", "MOD2"], writes=[("h2e", i2)])

        def body_A_load(k):
            e_, hf, i2 = k // 2, k % 2, k % 2
            S.dma("sp", lambda e: e.dma_start(out=hs_t[i2], in_=HS[e_ * CAP + hf * HCAP:e_ * CAP + (hf + 1) * HCAP, :].rearrange("(st p) d -> p st d", p=128)),
                  reads=["HS"], writes=[("hs_t", i2)], semkey=("hs_t", i2))

        def body_B(k):
            e_, i2 = k // 2, k % 2
            ws = e_ % NW
            for fc in range(2):
                for kc in range(8):
                    S.op("pe", lambda e, kc=kc, fc=fc: e.matmul(
                        bk(2)[:, fc * HCAP:(fc + 1) * HCAP], lhsT=wge[ws][:, kc, fc * 128:(fc + 1) * 128], rhs=h2e[i2][:, kc, :],
                        start=(kc == 0), stop=(kc == 7)),
                        reads=[("wge", ws), ("h2e", i2)], writes=[("bank", 2)])
                for kc in range(8):
                    S.op("pe", lambda e, kc=kc, fc=fc: e.matmul(
                        bk(3)[:, fc * HCAP:(fc + 1) * HCAP], lhsT=wue[ws][:, kc, fc * 128:(fc + 1) * 128], rhs=h2e[i2][:, kc, :],
                        start=(kc == 0), stop=(kc == 7)),
                        reads=[("wue", ws), ("h2e", i2)], writes=[("bank", 3)])
            S.op("act", lambda e: e.activation(out=sgs[i2], in_=bk(2), func=AF.Silu), reads=[("bank", 2)], writes=[("sgs", i2)])
            S.op("dve", lambda e: e.tensor_tensor(out=actT[i2].rearrange("p a b -> p (a b)"), in0=bk(3), in1=sgs[i2], op=ALU.mult),
                 reads=[("bank", 3), ("sgs", i2)], writes=[("actT", i2)])

        def body_C(k):
            e_, hf, i2 = k // 2, k % 2, k % 2
            ws = e_ % NW
            for st_ in range(2):
                yi = ysc[0] % 2
                ysc[0] += 1
                for dh in range(2):
                    pb = 4 + dh
                    for fc in range(2):
                        S.op("pe", lambda e, st_=st_, dh=dh, fc=fc, pb=pb: e.matmul(
                            bk(pb), lhsT=actT[i2][:, fc, st_ * 128:(st_ + 1) * 128], rhs=wde[ws][:, fc, dh * 512:(dh + 1) * 512],
                            start=(fc == 0), stop=(fc == 1)),
                            reads=[("actT", i2), ("wde", ws)], writes=[("bank", pb)])
                    if dh == 0:
                        S.op("act", lambda e, pb=pb, yi=yi: e.activation(out=ysb[yi][:, 0:512], in_=bk(pb), func=AF.Copy),
                             reads=[("bank", pb)], writes=[("ysb", yi)])
                    else:
                        S.op("dve", lambda e, pb=pb, yi=yi: e.tensor_copy(out=ysb[yi][:, 512:1024], in_=bk(pb)),
                             reads=[("bank", pb)], writes=[("ysb", yi)])
                S.dma("act", lambda e, st_=st_, yi=yi: e.dma_start(
                    out=YS[e_ * CAP + hf * HCAP + st_ * 128:e_ * CAP + hf * HCAP + (st_ + 1) * 128, :], in_=ysb[yi]),
                    reads=[("ysb", yi)], writes=[("YS", e_, hf, st_)], semkey=("YSw", yi))

        NBODY = 2 * NE
        body_A_load(0)
        body_A_load(1)
        stage_w(0)
        stage_w(1)
        casts_w(0)
        body_A(0)
        for k in range(NBODY):
            e_ = k // 2
            if k + 2 < NBODY:
                body_A_load(k + 2)
            if k % 2 == 0 and e_ >= 1:
                if e_ + 1 < NE:
                    stage_w(e_ + 1)
                casts_w(e_)
            if k + 1 < NBODY:
                body_A(k + 1)
            body_B(k)
            if k >= 1:
                body_C(k - 1)
        body_C(NBODY - 1)
        gc_ = 0
        for T in range(16):
            for k in range(2):
                gi = gc_ % len(gbuf)
                gc_ += 1
                S.dma("pool", lambda e, T=T, k=k, gi=gi: e.indirect_dma_start(
                    out=gbuf[gi], out_offset=None, in_=YS[:, :], in_offset=bass.IndirectOffsetOnAxis(ap=idx_i[:, T, k:k + 1], axis=0),
                    ),
                    reads=[("YS", e2, h2_, s2_) for e2 in range(NE) for h2_ in range(2) for s2_ in range(2)] + ["idx_i"], writes=[("gbuf", gi)], semkey=("gbuf", gi))
                S.op("dve", lambda e, T=T, k=k, gi=gi: e.scalar_tensor_tensor(
                    out=X1v(T), in0=gbuf[gi], scalar=wsel[:, T, k:k + 1], in1=X1v(T), op0=ALU.mult, op1=ALU.add),
                    reads=[("gbuf", gi), ("wsel", k), ("X1", T)], writes=[("X1", T)])
        S.barrier()
        sparse_top = A.top
        S = Sched(nc)
        S_dense = S
        subscheds.append(S)
        A.release(m5b)
        d_xn32 = A.alloc([1024], F32)
        d_h2f = [A.alloc([8, 128], F32) for _ in range(2)]
        d_h2T = XNB.rearrange("p a b -> p (a b)").rearrange("p (a b) -> p a b", a=8)
        d_actT = A.alloc([2, 2, TOWN], BF16)
        d_NW = 2
        d_wge = [A.alloc([8, 256], BF16) for _ in range(d_NW)]
        d_wue = [A.alloc([8, 256], BF16) for _ in range(d_NW)]
        d_wde = [A.alloc([2, 1024], BF16) for _ in range(d_NW)]
        d_combb = A.alloc([16, 32], BF16)
        d_combT = A.alloc([TOWN], BF16)
        d_SEL = A.alloc([32, 128], BF16)
        d_crs = [A.alloc([512], BF16) for _ in range(2)]
        d_sgs = [A.alloc([512], BF16) for _ in range(2)]
        d_tts = [A.alloc([512], BF16) for _ in range(2)]
        S.op("dve", lambda e: e.tensor_copy(out=d_SEL[0:32], in_=ident_b[0:32, 0:32].unsqueeze(2).to_broadcast([32, 32, 128])),
             reads=["ident_b"], writes=["SEL"])
        S.op("dve", lambda e: e.tensor_copy(out=d_combb, in_=comb), reads=["comb"], writes=["combb"])
        for T in range(16):
            rc_, rk = rstd_col(X1v(T), 128, [("X1", T)], D)
            S.op("dve", lambda e, T=T, rc_=rc_: e.tensor_scalar(out=d_xn32, in0=X1v(T), scalar1=rc_, scalar2=None, op0=ALU.mult),
                 reads=[("X1", T), rk], writes=["xn32"])
            hi = T % 2
            for half in range(2):
                pb = 0 + half
                for q4 in range(4):
                    kc = half * 4 + q4
                    S.op("pe", lambda e, kc=kc, q4=q4, pb=pb: e.transpose(out=bk(pb)[:, q4 * 128:(q4 + 1) * 128], in_=d_xn32[:, kc * 128:(kc + 1) * 128],
                                                                        identity=ident_f),
                         reads=["xn32", "ident_f"], writes=[("bank", pb)])
                for q4 in range(4):
                    kc = half * 4 + q4
                    S.op("act", lambda e, kc=kc, q4=q4, pb=pb, hi=hi: e.activation(
                        out=d_h2f[hi][:, kc, :], in_=bk(pb)[:, q4 * 128:(q4 + 1) * 128], func=AF.Identity, scale=G2[:, kc:kc + 1], bias=SH2[:, kc:kc + 1]),
                        reads=[("bank", pb), "G2", "MOD2"], writes=[("h2f", hi)])
            S.op("pool", lambda e, T=T, hi=hi: e.tensor_copy(out=d_h2T[:, :, T * 128:(T + 1) * 128], in_=d_h2f[hi]),
                 reads=[("h2f", hi)], writes=["h2T"])
        for T in range(16):
            pv = bkb(3)
            S.op("pe", lambda e, T=T, pv=pv: e.transpose(out=pv[0:32, 0:128], in_=d_combb[:, T, :], identity=ident_b),
                 reads=["combb", "ident_b"], writes=[("bank", 3)])
            S.op("act", lambda e, T=T, pv=pv: e.activation(out=d_combT[0:32, T * 128:(T + 1) * 128], in_=pv[0:32, 0:128], func=AF.Copy),
                 reads=[("bank", 3)], writes=["combT"])
        d_pgc = 0
        d_crc_ = [0]
        for e_ in range(NE):
            ws = e_ % d_NW
            es = e_ % 2
            S.dma("pool", lambda e, e_=e_, ws=ws: e.dma_start(out=d_wge[ws], in_=wg[e_].rearrange("(kc p) f -> p kc f", p=128)),
                  writes=[("wge", ws)], semkey=("wge", ws))
            S.dma("pool", lambda e, e_=e_, ws=ws: e.dma_start(out=d_wue[ws], in_=wu[e_].rearrange("(kc p) f -> p kc f", p=128)),
                  writes=[("wue", ws)], semkey=("wue", ws))
            S.dma("pool", lambda e, e_=e_, ws=ws: e.dma_start(out=d_wde[ws], in_=wd[e_].rearrange("(fc p) d -> p fc d", p=128)),
                  writes=[("wde", ws)], semkey=("wde", ws))
            for fc in range(2):
                S.op("dve", lambda e, ws=ws, fc=fc: e.tensor_tensor(out=d_wde[ws][:, fc, :], in0=d_wde[ws][:, fc, :], in1=GFrep, op=ALU.mult),
                     reads=[("wde", ws), ("Grep", id(GFrep), 0), ("Grep", id(GFrep), 1)], writes=[("wde", ws)])
            for tb in range(4):
                ci = d_crc_[0] % 2
                d_crc_[0] += 1
                S.op("pe", lambda e, e_=e_, tb=tb: e.matmul(bk(4), lhsT=d_SEL[0:32, e_, :], rhs=d_combT[0:32, tb * 512:(tb + 1) * 512], start=True, stop=True),
                     reads=["SEL", "combT"], writes=[("bank", 4)])
                S.op("act", lambda e, ci=ci: e.activation(out=d_crs[ci], in_=bk(4), func=AF.Copy), reads=[("bank", 4)], writes=[("crs", ci)])
                for fc in range(2):
                    pi = d_pgc % 2
                    d_pgc += 1
                    pg_, pu_ = 0 + 2 * pi, 1 + 2 * pi
                    for kc in range(8):
                        S.op("pe", lambda e, kc=kc, ws=ws, fc=fc, tb=tb, pg_=pg_: e.matmul(
                            bk(pg_), lhsT=d_wge[ws][:, kc, fc * 128:(fc + 1) * 128], rhs=d_h2T[:, kc, tb * 512:(tb + 1) * 512], start=(kc == 0), stop=(kc == 7)),
                            reads=[("wge", ws), "h2T"], writes=[("bank", pg_)])
                    for kc in range(8):
                        S.op("pe", lambda e, kc=kc, ws=ws, fc=fc, tb=tb, pu_=pu_: e.matmul(
                            bk(pu_), lhsT=d_wue[ws][:, kc, fc * 128:(fc + 1) * 128], rhs=d_h2T[:, kc, tb * 512:(tb + 1) * 512], start=(kc == 0), stop=(kc == 7)),
                            reads=[("wue", ws), "h2T"], writes=[("bank", pu_)])
                    S.op("act", lambda e, pi=pi, pg_=pg_: e.activation(out=d_sgs[pi], in_=bk(pg_), func=AF.Silu), reads=[("bank", pg_)], writes=[("sgs", pi)])
                    S.op("dve", lambda e, pi=pi, pu_=pu_: e.tensor_tensor(out=d_tts[pi], in0=bk(pu_), in1=d_sgs[pi], op=ALU.mult),
                         reads=[("bank", pu_), ("sgs", pi)], writes=[("tts", pi)])
                    S.op("pool", lambda e, pi=pi, ci=ci, es=es, fc=fc, tb=tb: e.tensor_tensor(
                        out=d_actT[:, es, fc, tb * 512:(tb + 1) * 512], in0=d_tts[pi], in1=d_crs[ci], op=ALU.mult),
                        reads=[("tts", pi), ("crs", ci)], writes=[("actT", es)])
            if e_ % 2 == 1:
                for T in range(16):
                    for dh in range(2):
                        pb = 5 + dh
                        n = 0
                        for es2 in range(2):
                            ws2 = (e_ - 1 + es2) % d_NW
                            for fc in range(2):
                                S.op("pe", lambda e, es2=es2, ws2=ws2, fc=fc, T=T, dh=dh, pb=pb, n=n: e.matmul(
                                    bk(pb), lhsT=d_actT[:, es2, fc, T * 128:(T + 1) * 128], rhs=d_wde[ws2][:, fc, dh * 512:(dh + 1) * 512],
                                    start=(n == 0), stop=(n == 3)),
                                    reads=[("actT", es2), ("wde", ws2)], writes=[("bank", pb)])
                                n += 1
                        S.op("dve", lambda e, T=T, dh=dh, pb=pb: e.tensor_tensor(
                            out=X1v(T)[:, dh * 512:(dh + 1) * 512], in0=bk(pb), in1=X1v(T)[:, dh * 512:(dh + 1) * 512], op=ALU.add),
                            reads=[("bank", pb), ("X1", T)], writes=[("X1", T)])
        S.barrier()
        A.top = max(A.top, sparse_top)
        S = S_main
        for eng_ in ENGS:
            def _branch(h, eng_=eng_):
                with h.register("moe_flag_" + eng_) as rg:
                    h.reg_load(rg, flag_i[0:1, 1:2])
                    with h.If_eq(rg, 0):
                        S_sparse._run(eng_, h)
                    with h.Else():
                        S_dense._run(eng_, h)
                return None
            S.op(eng_, _branch, reads=["flag_i"], no_ins=True)
        S.barrier()
        A.release(m5)
        FG = A.alloc([1024], F32)
        ot = [A.alloc([1024], F32) for _ in range(4)]
        ld(FG, gfin.partition_broadcast(128), "FG")
        for T in range(16):
            rc_, rk = rstd_col(X1v(T), 128, [("X1", T)], D)
            oi = T % 4
            S.op("dve", lambda e, T=T, rc_=rc_, oi=oi: e.scalar_tensor_tensor(out=ot[oi], in0=X1v(T), scalar=rc_, in1=FG, op0=ALU.mult, op1=ALU.mult),
                 reads=[("X1", T), rk, "FG"], writes=[("ot", oi)])
            S.dma("sp", lambda e, T=T, oi=oi: e.dma_start(out=out[T * 128:(T + 1) * 128, :], in_=ot[oi]),
                  reads=[("ot", oi)], writes=[("out", T)], semkey=("out", oi))
        return do_dumps_and_finish()


def prep_inputs(inputs):
    x = np.asarray(inputs["x"], np.float32)
    c = np.asarray(inputs["c"], np.float32)
    positions = np.asarray(inputs["positions"], np.int32)
    w_in = np.asarray(inputs["w_in"], np.float32)[0]
    idx = np.arange(512).reshape(8, 2, 32)[:, ::-1, :].reshape(512)
    winx = np.ascontiguousarray(np.concatenate([w_in, w_in[:, idx], w_in[:, 512 + idx]], axis=1))
    b_ada = np.asarray(inputs["b_ada"], np.float32)
    bchunks = b_ada[0].reshape(48, 128)
    b_fm = np.ascontiguousarray(bchunks[list(range(0, 16)) + list(range(24, 40))].T)
    fm = lambda v: np.ascontiguousarray(np.asarray(v, np.float32).reshape(8, 128).T)
    lamv = np.concatenate([np.asarray(inputs[k], np.float32)[0] for k in ("lambda_q1", "lambda_k1", "lambda_q2", "lambda_k2")])[None, :]
    cw = np.asarray(inputs["conv_w"], np.float32)[0]
    cwT = np.ascontiguousarray(cw.reshape(3, 4, 128).transpose(2, 1, 0).reshape(128, 12))
    wrt = np.ascontiguousarray(np.concatenate([np.asarray(inputs["w_group_router"], np.float32)[0],
                                               np.asarray(inputs["w_expert_router"], np.float32)[0]], axis=1))
    brt = np.concatenate([np.asarray(inputs["b_group_router"], np.float32)[0], np.asarray(inputs["b_expert_router"], np.float32)[0]])[None, :]
    d_ = np.arange(128) % 64
    invf = (10000.0 ** (-(d_ % 32).astype(np.float64) / 32.0)).astype(np.float32)[:, None]
    sgn = np.where(d_ < 32, -1.0, 1.0).astype(np.float32)[:, None]
    kidx = (np.arange(32)[None, :] * 128 + np.arange(128)[:, None]).astype(np.float32)
    shared = dict(
        w_ada=np.ascontiguousarray(np.asarray(inputs["w_ada"], np.float32)[0]), b_fm=b_fm, b_ada=np.ascontiguousarray(b_ada),
        gmix=fm(inputs["norm_mix_g"]), gffn=fm(inputs["norm_ffn_g"]), gfin=np.asarray(inputs["final_norm_g"], np.float32)[None, :],
        winx=winx, lamv=np.ascontiguousarray(lamv), subg=np.asarray(inputs["subln_g"], np.float32).reshape(1, 128),
        subgc=np.ascontiguousarray(np.asarray(inputs["subln_g"], np.float32).reshape(128, 1)), cwT=cwT,
        wua=np.ascontiguousarray(np.asarray(inputs["w_up_att"], np.float32)[0]),
        wuc=np.ascontiguousarray(np.asarray(inputs["w_up_conv"], np.float32)[0]),
        wout=np.ascontiguousarray(np.asarray(inputs["w_out"], np.float32)[0]), wrt=wrt, brt=np.ascontiguousarray(brt),
        wg=np.ascontiguousarray(np.asarray(inputs["w_gate"], np.float32)[0]), wu=np.ascontiguousarray(np.asarray(inputs["w_up"], np.float32)[0]),
        wd=np.ascontiguousarray(np.asarray(inputs["w_down"], np.float32)[0]), invf=invf, sgn=sgn, kidx=kidx,
        erow=np.arange(32, dtype=np.float32)[None, :],
        ut=(np.arange(128)[:, None] < np.arange(128)[None, :]).astype(np.float32))
    in_maps = []
    for cid in range(8):
        b, half = cid // 2, cid % 2
        chunks = OWN_CHUNKS[half]
        tok = np.concatenate([np.arange(cc * 512, (cc + 1) * 512) for cc in chunks])
        xhalo = np.zeros((8, D), np.float32)
        hvv = np.zeros((1, 8), np.float32)
        for i, cc in enumerate(chunks):
            if cc > 0:
                xhalo[2 * i:2 * i + 2] = x[b, cc * 512 - 2:cc * 512]
                hvv[0, 2 * i:2 * i + 2] = 1.0
        m = dict(shared)
        m.update(xo=np.ascontiguousarray(x[b, tok]), xs=np.ascontiguousarray(x[b]), xh=xhalo, hv=hvv,
                 cT=np.ascontiguousarray(c[b].reshape(8, 128).T), pos_s=np.ascontiguousarray(positions[b][None, :]),
                 pos_o=np.ascontiguousarray(positions[b, tok][None, :]), qidx=tok.astype(np.float32)[None, :])
        in_maps.append(m)
    return in_maps


_PROG = {}


def kernel(**inputs):
    in_maps = prep_inputs(inputs)
    if "full" not in _PROG:
        _PROG["full"] = build_program()
    nc = _PROG["full"]
    res = run_bass_kernel_spmd(nc, in_maps, core_ids=list(range(8)))
    outp = np.zeros((NB, SEQ, D), np.float32)
    for cid in range(8):
        b, half = cid // 2, cid % 2
        o = np.asarray(res.results[cid]["out"], np.float32)
        for i, cc in enumerate(OWN_CHUNKS[half]):
            outp[b, cc * 512:(cc + 1) * 512] = o[i * 512:(i + 1) * 512]
    return outp
```
